# Optimizing a Trainium2 kernel written in Bass

```python
import jax, jax.numpy as jnp
from jax import lax
import numpy as np


D_MODEL = 4096
BATCH = 2
SEQ = 8192
DEPTH = 2

HEAD_DIM = 128
N_ATTN_HEADS = D_MODEL // HEAD_DIM
NSA_HEADS = N_ATTN_HEADS // 2
NSA_KV_GROUPS = 4
NSA_GROUP_SIZE = NSA_HEADS // NSA_KV_GROUPS
NSA_CMP_STRIDE = 16
NSA_CMP_LEN = 2 * NSA_CMP_STRIDE
NSA_SEL_LEN = 64
NSA_N_SEL = 16
NSA_WINDOW = 512
NSA_QBLK = 32
MOBA_HEADS = N_ATTN_HEADS - NSA_HEADS
MOBA_BLOCK = 256
MOBA_TOPK = 3
MOBA_QBLK = 16
ATT_WIDTH = (NSA_HEADS + MOBA_HEADS) * HEAD_DIM

CONV_WIDTH = 3
CONV_CHANNELS = D_MODEL // 2
GLA_HEADS = 16
GLA_DK = 64
GLA_DV = 128
GLA_GATE_RANK = 16
GLA_GATE_TAU = 16.0
GLA_CHUNK = 64
MIX_WIDTH = CONV_CHANNELS + GLA_HEADS * GLA_DV

D_FF = 256 * ((8 * D_MODEL // 3 + 255) // 256)
ALPHA = (2 * DEPTH) ** 0.25
BETA = (8 * DEPTH) ** -0.25
LN_EPS = 1e-5
NORM_EPS = 1e-6
NEG_INF = -1e30
SEL_FORCE = 1e4

NSA_KV_DIM = NSA_KV_GROUPS * HEAD_DIM
L0_SPLITS = (NSA_HEADS * HEAD_DIM,) + (NSA_KV_DIM,) * 6 + (NSA_HEADS * 3,) + (MOBA_HEADS * HEAD_DIM,) * 3
L0_IN_DIM = sum(L0_SPLITS)
L1_SPLITS = (CONV_CHANNELS,) * 3 + (GLA_HEADS * GLA_DK,) * 2 + (GLA_HEADS * GLA_DV,) * 2 + (GLA_GATE_RANK,)
L1_IN_DIM = sum(L1_SPLITS)
N_EVEN = (DEPTH + 1) // 2
N_ODD = DEPTH // 2

kernel_name = 'hybrid_nsa_moba_shortconv_gla_macaron_deepnorm'


def _split(z, sizes):
    return jnp.split(z, np.cumsum(sizes)[:-1].tolist(), axis=-1)


def _layer_norm(x, g, b):
    xf = x.astype(jnp.float32)
    mu = jnp.mean(xf, axis=-1, keepdims=True)
    var = jnp.mean(jnp.square(xf - mu), axis=-1, keepdims=True)
    return ((xf - mu) * lax.rsqrt(var + LN_EPS) * g + b).astype(x.dtype)


def _swiglu(x, w_gate, w_up, w_down):
    return (jax.nn.silu(x @ w_gate) * (x @ w_up)) @ w_down


def _masked_softmax(s, mask):
    s = jnp.where(mask, s, NEG_INF)
    m = jnp.max(s, axis=-1, keepdims=True)
    p = jnp.where(mask, jnp.exp(s - m), 0.0)
    return p / jnp.maximum(jnp.sum(p, axis=-1, keepdims=True), 1e-30)


def _alibi_slopes(n):
    return jnp.exp2(-8.0 * jnp.arange(1, n + 1, dtype=jnp.float32) / n)


def _compress(k, pos, w1, w2):
    B, S, G, dh = k.shape
    ch = k.reshape(B, S // NSA_CMP_STRIDE, NSA_CMP_STRIDE, G, dh)
    blocks = jnp.concatenate([ch[:, :-1], ch[:, 1:]], axis=2) + pos[None, None, :, None, :]
    n_cmp = blocks.shape[1]
    flat = blocks.transpose(0, 1, 3, 2, 4).reshape(B, n_cmp, G, NSA_CMP_LEN * dh)
    return jax.nn.gelu(flat @ w1) @ w2


def _nsa(q, k_cmp, v_cmp, k_slc, v_slc, k_win, v_win, gates, slopes, pos_k, w1_k, w2_k, pos_v, w1_v, w2_v):
    B, S, G, R, dh = q.shape
    scale = dh ** -0.5
    f32 = jnp.float32
    kc = _compress(k_cmp, pos_k, w1_k, w2_k)
    vc = _compress(v_cmp, pos_v, w1_v, w2_v)
    n_cmp = kc.shape[1]
    cmp_start = jnp.arange(n_cmp) * NSA_CMP_STRIDE
    cmp_end = cmp_start + NSA_CMP_LEN - 1
    n_sel = S // NSA_SEL_LEN
    k_top = min(NSA_N_SEL, n_sel)
    sel_start = jnp.arange(n_sel) * NSA_SEL_LEN
    member = ((cmp_start[:, None] < sel_start[None, :] + NSA_SEL_LEN)
              & (cmp_start[:, None] + NSA_CMP_LEN > sel_start[None, :])).astype(f32)
    ks_blk = k_slc.reshape(B, n_sel, NSA_SEL_LEN, G, dh).transpose(0, 3, 1, 2, 4)
    vs_blk = v_slc.reshape(B, n_sel, NSA_SEL_LEN, G, dh).transpose(0, 3, 1, 2, 4)
    pad = ((0, 0), (NSA_WINDOW, 0), (0, 0), (0, 0))
    kw_pad = jnp.pad(k_win, pad)
    vw_pad = jnp.pad(v_win, pad)
    b_ix = jnp.arange(B)[:, None, None, None]
    g_ix = jnp.arange(G)[None, None, :, None]
    sel_ids = jnp.arange(n_sel)
    sl = slopes[None, None, :, :, None]
    T = NSA_QBLK

    def chunk(args):
        c, qc, gc = args
        t = c * T + jnp.arange(T)
        tf = t.astype(f32)
        s = jnp.einsum('btgrd,bngd->btgrn', qc, kc).astype(f32) * scale
        s = s - sl * (tf[:, None] - cmp_end[None, :].astype(f32))[None, :, None, None, :]
        p_cmp = _masked_softmax(s, (cmp_end[None, :] <= t[:, None])[None, :, None, None, :])
        o_cmp = jnp.einsum('btgrn,bngd->btgrd', p_cmp.astype(vc.dtype), vc)
        imp = jnp.einsum('btgn,nj->btgj', jnp.sum(p_cmp, axis=3), member)
        own = (t // NSA_SEL_LEN)[:, None]
        forced = (sel_ids == 0) | (sel_ids == own) | (sel_ids == own - 1)
        future = sel_ids > own
        imp = jnp.where(forced[None, :, None, :], imp + SEL_FORCE,
                        jnp.where(future[None, :, None, :], -1.0, imp))
        idx = lax.top_k(imp, k_top)[1]
        kg = ks_blk[b_ix, g_ix, idx].reshape(B, T, G, k_top * NSA_SEL_LEN, dh)
        vg = vs_blk[b_ix, g_ix, idx].reshape(B, T, G, k_top * NSA_SEL_LEN, dh)
        kpos = (idx[..., None] * NSA_SEL_LEN + jnp.arange(NSA_SEL_LEN)).reshape(B, T, G, 1, k_top * NSA_SEL_LEN)
        s = jnp.einsum('btgrd,btgkd->btgrk', qc, kg).astype(f32) * scale
        s = s - sl * (tf[None, :, None, None, None] - kpos.astype(f32))
        p = _masked_softmax(s, kpos <= t[None, :, None, None, None])
        o_slc = jnp.einsum('btgrk,btgkd->btgrd', p.astype(vg.dtype), vg)
        kw = lax.dynamic_slice_in_dim(kw_pad, c * T, NSA_WINDOW + T, axis=1)
        vw = lax.dynamic_slice_in_dim(vw_pad, c * T, NSA_WINDOW + T, axis=1)
        wpos = c * T - NSA_WINDOW + jnp.arange(NSA_WINDOW + T)
        wmask = (wpos[None, :] >= 0) & (wpos[None, :] <= t[:, None]) & (wpos[None, :] > t[:, None] - NSA_WINDOW)
        s = jnp.einsum('btgrd,bkgd->btgrk', qc, kw).astype(f32) * scale
        s = s - sl * (tf[:, None] - wpos[None, :].astype(f32))[None, :, None, None, :]
        p = _masked_softmax(s, wmask[None, :, None, None, :])
        o_win = jnp.einsum('btgrk,bkgd->btgrd', p.astype(vw.dtype), vw)
        return gc[..., 0:1] * o_cmp + gc[..., 1:2] * o_slc + gc[..., 2:3] * o_win

    n_q = S // T
    qch = q.reshape(B, n_q, T, G, R, dh).swapaxes(0, 1)
    gch = gates.reshape(B, n_q, T, G, R, 3).swapaxes(0, 1)
    out = lax.map(chunk, (jnp.arange(n_q), qch, gch))
    return out.swapaxes(0, 1).reshape(B, S, G * R * dh)


def _moba(q, k, v, slopes):
    B, S, H, dh = q.shape
    scale = dh ** -0.5
    f32 = jnp.float32
    n_blk = -(-S // MOBA_BLOCK)
    pad = ((0, 0), (0, n_blk * MOBA_BLOCK - S), (0, 0), (0, 0))
    k_blk = jnp.pad(k, pad).reshape(B, n_blk, MOBA_BLOCK, H, dh).transpose(0, 3, 1, 2, 4)
    v_blk = jnp.pad(v, pad).reshape(B, n_blk, MOBA_BLOCK, H, dh).transpose(0, 3, 1, 2, 4)
    k_mean = jnp.mean(k_blk, axis=3)
    n_top = min(MOBA_TOPK, n_blk - 1)
    n_gather = n_top + 1
    b_ix = jnp.arange(B)[:, None, None, None]
    h_ix = jnp.arange(H)[None, None, :, None]
    blk_ids = jnp.arange(n_blk)
    sl = slopes[None, None, :, None]
    T = MOBA_QBLK

    def chunk(args):
        c, qc = args
        t = c * T + jnp.arange(T)
        tf = t.astype(f32)
        own = jnp.broadcast_to((t // MOBA_BLOCK)[None, :, None, None], (B, T, H, 1))
        if n_top > 0:
            gate = jnp.einsum('bthd,bhnd->bthn', qc, k_mean).astype(f32)
            past = blk_ids < own
            gate = jnp.where(past, gate, NEG_INF)
            top = lax.top_k(gate, n_top)[1]
            idx = jnp.concatenate([top, own], axis=-1)
            valid = jnp.concatenate([top < own, jnp.ones_like(own, dtype=bool)], axis=-1)
        else:
            idx = own
            valid = jnp.ones_like(own, dtype=bool)
        kg = k_blk[b_ix, h_ix, idx].reshape(B, T, H, n_gather * MOBA_BLOCK, dh)
        vg = v_blk[b_ix, h_ix, idx].reshape(B, T, H, n_gather * MOBA_BLOCK, dh)
        kpos = (idx[..., None] * MOBA_BLOCK + jnp.arange(MOBA_BLOCK)).reshape(B, T, H, n_gather * MOBA_BLOCK)
        mask = jnp.repeat(valid, MOBA_BLOCK, axis=-1) & (kpos <= t[None, :, None, None])
        s = jnp.einsum('bthd,bthkd->bthk', qc, kg).astype(f32) * scale
        s = s - sl * (tf[None, :, None, None] - kpos.astype(f32))
        p = _masked_softmax(s, mask)
        return jnp.einsum('bthk,bthkd->bthd', p.astype(vg.dtype), vg)

    n_q = S // T
    qch = q.reshape(B, n_q, T, H, dh).swapaxes(0, 1)
    out = lax.map(chunk, (jnp.arange(n_q), qch))
    return out.swapaxes(0, 1).reshape(B, S, H * dh)


def _attn_mixer(x, w_in, w_out, pos_k, w1_k, w2_k, pos_v, w1_v, w2_v):
    B, S, _ = x.shape
    G, R, dh, H = NSA_KV_GROUPS, NSA_GROUP_SIZE, HEAD_DIM, MOBA_HEADS
    z = x @ w_in
    nq, kc, vc, ks, vs, kw, vw, ng, mq, mk, mv = _split(z, L0_SPLITS)
    kv = lambda a: a.reshape(B, S, G, dh)
    gates = jax.nn.sigmoid(ng).reshape(B, S, G, R, 3)
    slopes = _alibi_slopes(N_ATTN_HEADS)
    o_nsa = _nsa(nq.reshape(B, S, G, R, dh), kv(kc), kv(vc), kv(ks), kv(vs), kv(kw), kv(vw), gates,
                 slopes[0::2].reshape(G, R), pos_k, w1_k, w2_k, pos_v, w1_v, w2_v)
    o_moba = _moba(mq.reshape(B, S, H, dh), mk.reshape(B, S, H, dh), mv.reshape(B, S, H, dh), slopes[1::2])
    return jnp.concatenate([o_nsa, o_moba], axis=-1) @ w_out


def _short_conv(u, w):
    return lax.conv_general_dilated(u, w[:, None, :], window_strides=(1,), padding=((CONV_WIDTH - 1, 0),),
                                    dimension_numbers=('NWC', 'WIO', 'NWC'), feature_group_count=u.shape[-1])


def _gla(q, k, v, log_a, g_out, norm_g):
    B, S, H, dk = q.shape
    dv = v.shape[-1]
    f32 = jnp.float32
    L = GLA_CHUNK
    nc = S // L
    qf = q.astype(f32).reshape(B, nc, L, H, dk) * dk ** -0.5
    kf = k.astype(f32).reshape(B, nc, L, H, dk)
    vf = v.astype(f32).reshape(B, nc, L, H, dv)
    b = jnp.cumsum(log_a.reshape(B, nc, L, H, dk), axis=2)
    b_last = b[:, :, -1]
    q_t = qf * jnp.exp(b)
    k_t = kf * jnp.exp(-b)
    causal = jnp.tril(jnp.ones((L, L), dtype=bool))
    att = jnp.where(causal, jnp.einsum('bnihd,bnjhd->bnhij', q_t, k_t), 0.0)
    o_intra = jnp.einsum('bnhij,bnjhe->bnihe', att, vf)
    u = jnp.einsum('bnjhd,bnjhe->bnhde', kf * jnp.exp(b_last[:, :, None] - b), vf)
    decay = jnp.exp(b_last)

    def step(state, inp):
        dec, uc = inp
        return dec[..., None] * state + uc, state

    state0 = jnp.zeros((B, H, dk, dv), f32)
    _, s_prev = lax.scan(step, state0, (jnp.moveaxis(decay, 1, 0), jnp.moveaxis(u, 1, 0)))
    s_prev = jnp.moveaxis(s_prev, 0, 1)
    o = o_intra + jnp.einsum('bnihd,bnhde->bnihe', q_t, s_prev)
    o = o.reshape(B, S, H, dv)
    o = o * lax.rsqrt(jnp.mean(jnp.square(o), axis=-1, keepdims=True) + NORM_EPS) * norm_g
    return o.astype(v.dtype).reshape(B, S, H * dv) * jax.nn.silu(g_out)


def _conv_gla_mixer(x, w_in, w_out, conv_w, w_a2, b_a, norm_g):
    B, S, _ = x.shape
    H, dk, dv = GLA_HEADS, GLA_DK, GLA_DV
    z = x @ w_in
    gb, gc, h, q, k, v, g, za = _split(z, L1_SPLITS)
    y_conv = gb * _short_conv(gc * h, conv_w)
    log_a = jax.nn.log_sigmoid((za @ w_a2 + b_a).astype(jnp.float32)) / GLA_GATE_TAU
    y_gla = _gla(q.reshape(B, S, H, dk), k.reshape(B, S, H, dk), v.reshape(B, S, H, dv),
                 log_a.reshape(B, S, H, dk), g, norm_g)
    return jnp.concatenate([y_conv, y_gla], axis=-1) @ w_out


def setup_inputs(seed: int = 0) -> dict:
    key = jax.random.key(seed)
    keys = iter(jax.random.split(key, 32))
    f32 = jnp.float32

    def nrm(shape, scale):
        return jax.random.normal(next(keys), shape, f32) * scale

    dh = HEAD_DIM
    cmp_in = NSA_CMP_LEN * dh
    return {
        'x': nrm((BATCH, SEQ, D_MODEL), 1.0),
        'ln_g': 1.0 + nrm((DEPTH, 3, D_MODEL), 0.01),
        'ln_b': nrm((DEPTH, 3, D_MODEL), 0.01),
        'ffn_pre_wg': nrm((DEPTH, D_MODEL, D_FF), D_MODEL ** -0.5),
        'ffn_pre_wu': nrm((DEPTH, D_MODEL, D_FF), D_MODEL ** -0.5),
        'ffn_pre_wd': nrm((DEPTH, D_FF, D_MODEL), BETA * D_FF ** -0.5),
        'ffn_post_wg': nrm((DEPTH, D_MODEL, D_FF), D_MODEL ** -0.5),
        'ffn_post_wu': nrm((DEPTH, D_MODEL, D_FF), D_MODEL ** -0.5),
        'ffn_post_wd': nrm((DEPTH, D_FF, D_MODEL), BETA * D_FF ** -0.5),
        'att_w_in': nrm((N_EVEN, D_MODEL, L0_IN_DIM), D_MODEL ** -0.5),
        'att_w_out': nrm((N_EVEN, ATT_WIDTH, D_MODEL), BETA * ATT_WIDTH ** -0.5),
        'nsa_pos_k': nrm((N_EVEN, NSA_CMP_LEN, dh), 0.1),
        'nsa_w1_k': nrm((N_EVEN, cmp_in, dh), cmp_in ** -0.5),
        'nsa_w2_k': nrm((N_EVEN, dh, dh), dh ** -0.5),
        'nsa_pos_v': nrm((N_EVEN, NSA_CMP_LEN, dh), 0.1),
        'nsa_w1_v': nrm((N_EVEN, cmp_in, dh), cmp_in ** -0.5),
        'nsa_w2_v': nrm((N_EVEN, dh, dh), dh ** -0.5),
        'mix_w_in': nrm((N_ODD, D_MODEL, L1_IN_DIM), D_MODEL ** -0.5),
        'mix_w_out': nrm((N_ODD, MIX_WIDTH, D_MODEL), BETA * MIX_WIDTH ** -0.5),
        'conv_w': nrm((N_ODD, CONV_WIDTH, CONV_CHANNELS), CONV_WIDTH ** -0.5),
        'gla_w_a2': nrm((N_ODD, GLA_GATE_RANK, GLA_HEADS * GLA_DK), GLA_GATE_RANK ** -0.5),
        'gla_b_a': nrm((N_ODD, GLA_HEADS * GLA_DK), 0.1),
        'gla_norm_g': 1.0 + nrm((N_ODD, GLA_DV), 0.01),
    }


def reference(x, ln_g, ln_b, ffn_pre_wg, ffn_pre_wu, ffn_pre_wd, ffn_post_wg, ffn_post_wu, ffn_post_wd,
              att_w_in, att_w_out, nsa_pos_k, nsa_w1_k, nsa_w2_k, nsa_pos_v, nsa_w1_v, nsa_w2_v,
              mix_w_in, mix_w_out, conv_w, gla_w_a2, gla_b_a, gla_norm_g):
    for layer in range(DEPTH):
        h = _swiglu(x, ffn_pre_wg[layer], ffn_pre_wu[layer], ffn_pre_wd[layer])
        x = _layer_norm(ALPHA * x + 0.5 * h, ln_g[layer, 0], ln_b[layer, 0])
        i = layer // 2
        if layer % 2 == 0:
            y = _attn_mixer(x, att_w_in[i], att_w_out[i], nsa_pos_k[i], nsa_w1_k[i], nsa_w2_k[i],
                            nsa_pos_v[i], nsa_w1_v[i], nsa_w2_v[i])
        else:
            y = _conv_gla_mixer(x, mix_w_in[i], mix_w_out[i], conv_w[i], gla_w_a2[i], gla_b_a[i], gla_norm_g[i])
        x = _layer_norm(ALPHA * x + y, ln_g[layer, 1], ln_b[layer, 1])
        h = _swiglu(x, ffn_post_wg[layer], ffn_post_wu[layer], ffn_post_wd[layer])
        x = _layer_norm(ALPHA * x + 0.5 * h, ln_g[layer, 2], ln_b[layer, 2])
    return x
```

```python
import math
import numpy as np
import concourse.bass as bass
import concourse.mybir as mybir
from concourse.bass_utils import run_bass_kernel_spmd

F32 = mybir.dt.float32
BF16 = mybir.dt.bfloat16
AF = mybir.ActivationFunctionType
ALU = mybir.AluOpType
AX = mybir.AxisListType

D_MODEL = 4096
BATCH = 2
SEQ = 8192
DEPTH = 2
D_FF = 11008
ALPHA = (2 * DEPTH) ** 0.25
LN_EPS = 1e-5
NCORES = 8


class Buf:
    __slots__ = ("name", "w", "rs", "dsem", "dcnt")

    def __init__(self, name):
        self.name = name
        self.w = None
        self.rs = {}
        self.dsem = None
        self.dcnt = 0


class Ctx:
    def __init__(self, nc):
        self.nc = nc
        self.E = {"pe": nc.tensor, "act": nc.scalar, "dve": nc.vector, "pool": nc.gpsimd, "sp": nc.sync}
        self.esem = {e: nc.alloc_semaphore("es_" + e) for e in self.E}
        self.ecnt = {e: 0 for e in self.E}
        self.seen = {e: {} for e in self.E}
        self.semcnt = {}
        self.nbuf = 0

    def buf(self, name=None):
        self.nbuf += 1
        return Buf(name or "b%d" % self.nbuf)

    def bufs(self, n, name="b"):
        return [self.buf("%s%d" % (name, i)) for i in range(n)]

    def _wait(self, e, tok):
        sem, val, own = tok
        if own == e and e in ("pe", "sp"):
            return
        if own == e and val > self.ecnt[e]:
            return
        if sem.num in self.semcnt:
            val = max(val, self.semcnt[sem.num])
        if self.seen[e].get(sem.num, 0) >= val:
            return
        self.E[e].wait_ge(sem, val)
        self.seen[e][sem.num] = val

    def _deps(self, e, reads, writes):
        for b in reads:
            if b.w is not None:
                self._wait(e, b.w)
        for b in writes:
            if b.w is not None:
                self._wait(e, b.w)
            for t in b.rs.values():
                self._wait(e, t)

    def _record(self, tok, reads, writes):
        for b in reads:
            b.rs[tok[0].num] = tok
        for b in writes:
            b.w = tok
            b.rs = {}

    def op(self, e, fn, reads=(), writes=(), inc=True):
        self._deps(e, reads, writes)
        ins = fn(self.E[e])
        if inc:
            self.ecnt[e] += 1
            ins.then_inc(self.esem[e], 1)
            tok = (self.esem[e], self.ecnt[e], e)
        else:
            tok = (self.esem[e], self.ecnt[e] + 1, e)
        self._record(tok, reads, writes)
        return ins

    def dma(self, q, out, in_, sb, reads=(), writes=()):
        self._deps(q, reads, writes)
        if sb.dsem is None:
            sb.dsem = self.nc.alloc_semaphore("ds_" + sb.name)
        ins = self.E[q].dma_start(out=out, in_=in_)
        sb.dcnt += 16
        ins.then_inc(sb.dsem, 16)
        self.semcnt[sb.dsem.num] = sb.dcnt
        tok = (sb.dsem, sb.dcnt, "dma")
        self._record(tok, reads, writes)
        return ins

    def finish(self, bufs):
        for b in bufs:
            if b.w is not None:
                self._wait("sp", b.w)


def mm(cx, out_ap, lhsT, rhs, start, stop, reads, writes):
    cx.op("pe", lambda e: e.matmul(out_ap, lhsT, rhs, start=start, stop=stop), reads, writes, inc=stop)


def build_dense(mode, D, F, T, TG, NOUT=None):
    nc = bass.Bass("TRN2", target_bir_lowering=False)
    cx = Ctx(nc)
    KD = D // 128
    NG = T // TG
    dram = {}

    def din(name, shape, dt=F32):
        dram[name] = nc.dram_tensor(name, list(shape), dt, kind="ExternalInput").ap()
        return dram[name]

    xT = din("xT", [D, T])
    if mode == "ffn":
        KF = F // 128
        wg = din("wg", [KF, 128, KD * 128])
        wu = din("wu", [KF, 128, KD * 128])
        wd = din("wd", [KD, 128, KF * 128])
        KB = KF
    elif mode == "out":
        oT = din("oT", [D, T])
        wd = din("wd", [KD, 128, KD * 128])
        KB = KD
    else:
        NT = NOUT // 128
        wl = din("wl", [NT, 128, KD * 128])
    if mode != "lin":
        lng = din("lng", [128, KD])
        lnb = din("lnb", [128, KD])
        yT = nc.dram_tensor("yT", [D, T], F32, kind="ExternalOutput").ap()
        vscr = nc.dram_tensor("vscr", [D, T], F32, kind="Internal").ap()
        res_c = (0.5 / ALPHA) if mode == "ffn" else (1.0 / ALPHA)
        eps = LN_EPS / (ALPHA * ALPHA)
    else:
        yT = nc.dram_tensor("yT", [NOUT, T], F32, kind="ExternalOutput").ap()
    yT_b = cx.buf("yT")
    vscr_b = cx.buf("vscr")

    xb = nc.alloc_sbuf_tensor("xb", [128, KD, TG], BF16)
    xb_b = cx.buf("xb")
    if mode == "lin":
        WSZ = KD * 128
    else:
        WSZ = max(KB * 128, 2 * KD * 128 if mode == "ffn" else 0)
    wsl = [nc.alloc_sbuf_tensor("w%d" % i, [128, WSZ], BF16) for i in range(2)]
    wsl_b = [cx.buf("w%d" % i) for i in range(2)]
    if mode != "lin":
        hT = nc.alloc_sbuf_tensor("hT", [128, KB, TG], BF16)
        hT_b = cx.bufs(KB, "hT")
        ones = nc.alloc_sbuf_tensor("ones", [128, 128], F32)
        ones_b = cx.buf("ones")
        lng_s = nc.alloc_sbuf_tensor("lng_s", [128, KD], F32)
        lnb_s = nc.alloc_sbuf_tensor("lnb_s", [128, KD], F32)
        ln_b = cx.buf("ln")
        cx.op("dve", lambda e: e.memset(ones[:, :], 1.0), [], [ones_b])
        cx.dma("sp", lng_s[:, :], lng, ln_b, [], [ln_b])
        cx.dma("sp", lnb_s[:, :], lnb, ln_b, [], [ln_b])

    def tiles(name, n, dt=F32, w=None):
        w = w or TG
        return ([nc.alloc_sbuf_tensor("%s%d" % (name, i), [128, w], dt) for i in range(n)], cx.bufs(n, name))

    sg, sg_b = tiles("sg", 2)
    if mode != "lin":
        xr, xr_b = tiles("xr", 2)
        vt, vt_b = tiles("vt", 2)
        sq, sq_b = tiles("sq", 2)
        t1, t1_b = tiles("t1", 2)
        yt, yt_b = tiles("yt", 2)
        st, st_b = tiles("st", 4)
    ps = [nc.alloc_psum_tensor("ps%d" % i, [128, 512], F32) for i in range(8)]
    ps_b = cx.bufs(8, "ps")

    xT_v = xT.rearrange("(k p) t -> p k t", p=128)
    if mode == "out":
        oT_v = oT.rearrange("(k p) t -> p k t", p=128)
    wcount = [0]

    def wslot():
        s = wcount[0] % 2
        wcount[0] += 1
        return s

    for g in range(NG):
        tsl = slice(g * TG, (g + 1) * TG)
        if mode in ("ffn", "lin"):
            cx.dma("pool", xb[:, :, :], xT_v[:, :, tsl], xb_b, [], [xb_b])
        if mode == "ffn":
            for ft in range(KF):
                s = wslot()
                cx.dma("pool", wsl[s][:, 0:KD * 128], wg[ft], wsl_b[s], [], [wsl_b[s]])
                cx.dma("pool", wsl[s][:, KD * 128:2 * KD * 128], wu[ft], wsl_b[s], [], [wsl_b[s]])
                pa, pb = ft % 2, 2 + ft % 2
                for k in range(KD):
                    mm(cx, ps[pa][:, 0:TG], wsl[s][:, k * 128:(k + 1) * 128], xb[:, k, :], k == 0, k == KD - 1,
                       [wsl_b[s], xb_b], [ps_b[pa]])
                for k in range(KD):
                    mm(cx, ps[pb][:, 0:TG], wsl[s][:, (KD + k) * 128:(KD + k + 1) * 128], xb[:, k, :], k == 0,
                       k == KD - 1, [wsl_b[s], xb_b], [ps_b[pb]])
                j = ft % 2
                cx.op("act", lambda e: e.activation(out=sg[j][:, :], in_=ps[pa][:, 0:TG], func=AF.Silu),
                      [ps_b[pa]], [sg_b[j]])
                cx.op("dve", lambda e: e.tensor_tensor(out=hT[:, ft, :], in0=sg[j][:, :], in1=ps[pb][:, 0:TG],
                                                       op=ALU.mult), [sg_b[j], ps_b[pb]], [hT_b[ft]])
        elif mode == "lin":
            for nt in range(NT):
                s = wslot()
                cx.dma("pool", wsl[s][:, 0:KD * 128], wl[nt], wsl_b[s], [], [wsl_b[s]])
                pa = nt % 2
                for k in range(KD):
                    mm(cx, ps[pa][:, 0:TG], wsl[s][:, k * 128:(k + 1) * 128], xb[:, k, :], k == 0, k == KD - 1,
                       [wsl_b[s], xb_b], [ps_b[pa]])
                j = nt % 2
                cx.op("act", lambda e: e.activation(out=sg[j][:, :], in_=ps[pa][:, 0:TG], func=AF.Copy),
                      [ps_b[pa]], [sg_b[j]])
                cx.dma("sp", yT[nt * 128:(nt + 1) * 128, tsl], sg[j][:, :], sg_b[j], [sg_b[j]], [yT_b])
            continue
        else:
            cx.dma("pool", hT[:, :, :], oT_v[:, :, tsl], hT_b[0], [], hT_b)
        for dt_ in range(KD):
            s = wslot()
            cx.dma("pool", wsl[s][:, 0:KB * 128], wd[dt_], wsl_b[s], [], [wsl_b[s]])
            j = dt_ % 2
            cx.dma("sp", xr[j][:, :], xT[dt_ * 128:(dt_ + 1) * 128, tsl], xr_b[j], [], [xr_b[j]])
            po = 4 + j
            for k in range(KB):
                mm(cx, ps[po][:, 0:TG], wsl[s][:, k * 128:(k + 1) * 128], hT[:, k, :], k == 0, k == KB - 1,
                   [wsl_b[s], hT_b[k]], [ps_b[po]])
            cx.op("dve", lambda e: e.scalar_tensor_tensor(out=vt[j][:, :], in0=ps[po][:, 0:TG], scalar=res_c,
                                                          in1=xr[j][:, :], op0=ALU.mult, op1=ALU.add),
                  [ps_b[po], xr_b[j]], [vt_b[j]])
            cx.op("act", lambda e: e.activation(out=sq[j][:, :], in_=vt[j][:, :], func=AF.Square),
                  [vt_b[j]], [sq_b[j]])
            mm(cx, ps[6][:, 0:TG], ones[:, :], vt[j][:, :], dt_ == 0, dt_ == KD - 1, [ones_b, vt_b[j]], [ps_b[6]])
            mm(cx, ps[7][:, 0:TG], ones[:, :], sq[j][:, :], dt_ == 0, dt_ == KD - 1, [ones_b, sq_b[j]], [ps_b[7]])
            cx.dma("sp", vscr[dt_ * 128:(dt_ + 1) * 128, tsl], vt[j][:, :], vt_b[j], [vt_b[j]], [vscr_b])
        mean, var, rstd, nmr = st
        mean_b, var_b, rstd_b, nmr_b = st_b
        cx.op("dve", lambda e: e.tensor_scalar(out=mean[:, :], in0=ps[6][:, 0:TG], scalar1=1.0 / D, scalar2=None,
                                               op0=ALU.mult), [ps_b[6]], [mean_b])
        cx.op("dve", lambda e: e.tensor_tensor(out=var[:, :], in0=mean[:, :], in1=mean[:, :], op=ALU.mult),
              [mean_b], [var_b])
        cx.op("dve", lambda e: e.scalar_tensor_tensor(out=var[:, :], in0=ps[7][:, 0:TG], scalar=1.0 / D,
                                                      in1=var[:, :], op0=ALU.mult, op1=ALU.subtract),
              [ps_b[7], var_b], [var_b])
        cx.op("dve", lambda e: e.tensor_scalar(out=var[:, :], in0=var[:, :], scalar1=eps, scalar2=None,
                                               op0=ALU.add), [var_b], [var_b])
        cx.op("act", lambda e: e.activation(out=var[:, :], in_=var[:, :], func=AF.Sqrt), [var_b], [var_b])
        cx.op("dve", lambda e: e.reciprocal(out=rstd[:, :], in_=var[:, :]), [var_b], [rstd_b])
        cx.op("dve", lambda e: e.scalar_tensor_tensor(out=nmr[:, :], in0=mean[:, :], scalar=-1.0, in1=rstd[:, :],
                                                      op0=ALU.mult, op1=ALU.mult), [mean_b, rstd_b], [nmr_b])
        for dt_ in range(KD):
            j = dt_ % 2
            cx.dma("sp", vt[j][:, :], vscr[dt_ * 128:(dt_ + 1) * 128, tsl], vt_b[j], [vscr_b], [vt_b[j]])
            cx.op("dve", lambda e: e.tensor_tensor(out=t1[j][:, :], in0=vt[j][:, :], in1=rstd[:, :], op=ALU.mult),
                  [vt_b[j], rstd_b], [t1_b[j]])
            cx.op("dve", lambda e: e.tensor_tensor(out=t1[j][:, :], in0=t1[j][:, :], in1=nmr[:, :], op=ALU.add),
                  [t1_b[j], nmr_b], [t1_b[j]])
            cx.op("act", lambda e: e.activation(out=yt[j][:, :], in_=t1[j][:, :], func=AF.Identity,
                                                bias=lnb_s[:, dt_:dt_ + 1], scale=lng_s[:, dt_:dt_ + 1]),
                  [t1_b[j], ln_b], [yt_b[j]])
            cx.dma("sp", yT[dt_ * 128:(dt_ + 1) * 128, tsl], yt[j][:, :], yt_b[j], [yt_b[j]], [yT_b])
    cx.finish([yT_b])
    return nc


def tile_w(w, ncols_pad=None):
    K, N = w.shape
    if ncols_pad is not None and ncols_pad != N:
        wp = np.zeros((K, ncols_pad), w.dtype)
        wp[:, :N] = w
        w = wp
        N = ncols_pad
    a = w.reshape(K // 128, 128, N // 128, 128)
    return np.ascontiguousarray(a.transpose(2, 1, 0, 3)).reshape(N // 128, 128, (K // 128) * 128)


def ln_tile(v):
    return np.ascontiguousarray(v.reshape(-1, 128).T)


NEG = -30000.0
HD = 128
ATT_SCALE = HD ** -0.5


def attn_consts(S, slopes_nsa, slopes_moba):
    import ml_dtypes
    bf = ml_dtypes.bfloat16
    NQ = S // 512
    c = {}
    sl = np.concatenate([slopes_nsa, slopes_moba]).astype(np.float64)
    f = np.arange(512, dtype=np.float64)
    cr = -(sl[:, None] * f[None, :]) / ATT_SCALE
    hi = cr.astype(bf).astype(np.float64)
    lo = (cr - hi).astype(bf).astype(np.float64)
    c["crow"] = np.stack([hi, lo], axis=1).astype(np.float32)
    p = np.arange(128, dtype=np.float64)
    r = np.arange(-3, 61, dtype=np.float64)
    c["ab"] = (sl[None, :, None] * (p[:, None, None] - 128.0 * r[None, None, :])).astype(np.float32)
    nt = np.arange(4)
    q = np.arange(NQ)
    c["abc"] = (sl[None, :4, None, None] * (16.0 * (128.0 * nt[None, None, :, None] + p[:, None, None, None]) + 31.0
                                            - 512.0 * q[None, None, None, :])).astype(np.float32).reshape(128, 4, 4 * NQ)
    P = p[:, None]
    Fq = f[None, :]
    masks = []
    for rr in (0, -1, -2, -3):
        masks.append(np.where(P - Fq - 128 * rr <= 0, 0.0, NEG))
    for rr in (1, 2, 3, 4):
        masks.append(np.where(P - Fq - 128 * rr > -512, 0.0, NEG))
    for e in range(5):
        masks.append(np.where(16 * P + 31 - 512 * e <= Fq, 0.0, NEG))
    c["masks"] = np.stack(masks, axis=1).astype(np.float32)
    NSEL = S // 64
    NKT = S // 128
    E = np.zeros((NSEL, NKT, 128), np.float32)
    for kt in range(NKT):
        E[2 * kt, kt, :64] = 1
        E[2 * kt + 1, kt, 64:] = 1
    c["ensa"] = E
    NB = S // 256
    Em = np.zeros((NB + 2, NB, 128), np.float32)
    for b in range(NB):
        Em[b, b, :] = 1
    Em[NB:, :, :] = 1
    c["emoba"] = Em
    ncmp = S // 16 - 1
    n = np.arange(((ncmp + 127) // 128) * 128)
    j = np.arange(NSEL)
    mem = ((16 * n[:, None] < 64 * j[None, :] + 64) & (16 * n[:, None] + 32 > 64 * j[None, :]) & (n[:, None] < ncmp))
    c["member"] = np.ascontiguousarray(mem.astype(np.float32).reshape(-1, 128, NSEL).transpose(1, 0, 2))
    cc = np.arange(256)[None, :]
    hp = (np.arange(128)[:, None] >= 64).astype(np.int64)
    fut = (cc - 128 > hp).astype(np.float32)
    own = ((cc - 128 - hp == 0) | (cc - 128 - hp == -1)).astype(np.float32)
    c["selk"] = np.stack([1.0 - fut, fut, 1e4 * own], axis=1).astype(np.float32)
    c["ident"] = np.eye(128, dtype=np.float32)
    gs = np.zeros((12, 12, 128), np.float32)
    for k in range(12):
        gs[k, k, :] = 1
    c["gsel"] = gs
    return c


def build_attn(S):
    nc = bass.Bass("TRN2", target_bir_lowering=False)
    cx = Ctx(nc)
    NQ = S // 512
    NKT = S // 128
    NSEL = S // 64
    NB = S // 256
    NCMP = S // 16 - 1
    NCT = (NCMP + 127) // 128
    GW = max(NB, 8)

    def din(name, shape):
        return nc.dram_tensor(name, list(shape), F32, kind="ExternalInput").ap()

    nqT = din("nqT", [4, 128, S]); kcT = din("kcT", [128, S]); vcT = din("vcT", [128, S])
    ksT = din("ksT", [128, S]); vs = din("vs", [S, 128]); kwT = din("kwT", [128, S]); vw = din("vw", [S, 128])
    ngT = din("ngT", [12, S])
    mqT = din("mqT", [4, 128, S]); mkT = din("mkT", [4, 128, S]); mv = din("mv", [4, S, 128])
    w1 = [din("w1k", [128, 32 * 128]), din("w1v", [128, 32 * 128])]
    posT = [din("poskT", [128, 32]), din("posvT", [128, 32])]
    w2 = [din("w2k", [128, 128]), din("w2v", [128, 128])]
    crow = din("crow", [8, 2, 512]); ab_d = din("ab", [128, 8, 64]); abc_d = din("abc", [128, 4, 4 * NQ])
    masks_d = din("masks", [128, 13, 512]); ensa_d = din("ensa", [NSEL, NKT, 128]); emoba_d = din("emoba", [NB + 2, NB, 128])
    member_d = din("member", [128, NCT, NSEL]); selk_d = din("selk", [128, 3, 256]); ident_d = din("ident", [128, 128])
    gsel_d = din("gsel", [12, 12, 128])
    oT = nc.dram_tensor("oT", [8 * 128, S], F32, kind="ExternalOutput").ap()
    oacc = nc.dram_tensor("oacc", [4 * 128, S], F32, kind="Internal").ap()
    oT_b = cx.buf("oT"); oacc_b = cx.buf("oacc")

    def sb(name, shape, dt=F32):
        return nc.alloc_sbuf_tensor("s_" + name, list(shape), dt), cx.buf(name)

    QB = [sb("qb%d" % i, [128, S], BF16) for i in range(2)]
    QT = [sb("qt%d" % i, [128, 512], BF16) for i in range(2)]
    KV = [sb("kv%d" % i, [128, S], BF16) for i in range(4)]
    AT, AT_b = sb("AT", [128, max(S, 4096)], BF16)
    EB, EB_b = sb("EB", [128, NKT * 128], BF16)
    masks, masks_b = sb("masks", [128, 13, 512], BF16)
    ab, ab_b = sb("ab", [128, 8, 64]); abc, abc_b = sb("abc", [128, 4, 4 * NQ])
    member, member_b = sb("member", [128, NCT, NSEL], BF16)
    selk, selk_b = sb("selk", [128, 3, 256])
    ident, ident_b = sb("ident", [128, 128], BF16)
    onesb, onesb_b = sb("onesb", [128, 128], BF16)
    gsel, gsel_b = sb("gsel", [12, 12 * 128])
    NGQ = [sb("ngq%d" % i, [12, 512]) for i in range(2)]
    crw, crw_b = sb("crw", [2, 8, 512], BF16)
    pT = [sb("pT%d" % i, [128, 512], BF16) for i in range(4)]
    fA = [sb("fA%d" % i, [128, 512]) for i in range(2)]
    fB = [sb("fB%d" % i, [128, 512]) for i in range(2)]
    fC = [sb("fC%d" % i, [128, 512]) for i in range(2)]
    fD = [sb("fD%d" % i, [128, 512]) for i in range(2)]
    s1, s1_b = sb("s1", [128, 128]); s2, s2_b = sb("s2", [128, 128])
    m8a, m8a_b = sb("m8a", [128, 8]); m8b, m8b_b = sb("m8b", [128, 8])
    mbt, mbt_b = sb("mbt", [128, 128], BF16)
    kcc = [sb("kcc%d" % i, [128, 512], BF16) for i in range(2)]
    hid, hid_b = sb("hid", [128, 512], BF16)
    cb, cb_b = sb("cb", [128, 2])
    pw2 = [sb("w2_%d" % i, [128, 128], BF16) for i in range(2)]
    pposT = [sb("posT%d" % i, [128, 32], BF16) for i in range(2)]
    kmT, kmT_b = sb("kmT", [128, 32]); kmTb, kmTb_b = sb("kmTb", [128, 32], BF16)
    U3, U3_b = sb("U3", [128, 3, 64])
    PS = [(nc.alloc_psum_tensor("ps%d" % i, [128, 512], F32), cx.buf("ps%d" % i)) for i in range(8)]
    ST = PS[0:2]; OACC = PS[2]; RS = PS[3]; IMP = PS[4]; MISC = PS[5:8]
    misc_i = [0]

    def misc():
        misc_i[0] += 1
        return MISC[misc_i[0] % 3]

    def ld(dst, dst_b, src, q="pool"):
        cx.dma(q, dst, src, dst_b, [], [dst_b])

    ld(masks[:, :, :], masks_b, masks_d)
    ld(ab[:, :, :], ab_b, ab_d, "sp"); ld(abc[:, :, :], abc_b, abc_d, "sp")
    ld(member[:, :, :], member_b, member_d); ld(selk[:, :, :], selk_b, selk_d, "sp")
    ld(ident[:, :], ident_b, ident_d); ld(gsel[:, :], gsel_b, gsel_d.rearrange("k r m -> k (r m)"), "sp")
    ld(crw[:, :, :], crw_b, crow.rearrange("h r f -> r h f"))
    cx.op("dve", lambda e: e.memset(onesb[:, :], 1.0), [], [onesb_b])
    cx.op("dve", lambda e: e.memset(U3[:, :, :], 0.0), [], [U3_b])
    cx.op("dve", lambda e: e.memset(U3[:, 0, 0:32], 1.0), [], [U3_b])
    cx.op("dve", lambda e: e.memset(U3[:, 1, 32:33], 1.0), [], [U3_b])
    cx.op("dve", lambda e: e.memset(U3[:, 2, 32:64], -1e30), [], [U3_b])

    def exp_tile(st, st_b, kp, bias_ap, bias_b, slot):
        p, p_b = pT[slot % 4]
        cx.op("act", lambda e: e.activation(out=p[:kp, :], in_=st[:kp, :], func=AF.Exp, bias=bias_ap, scale=ATT_SCALE),
              [st_b, bias_b], [p_b])
        return p, p_b

    cnt = {"pair": 0, "it": 0}

    def attn_pairs(q_rhs, q_b, pairs):
        kept = []
        n = len(pairs)
        for i, pr in enumerate(pairs):
            st, st_b = ST[cnt["pair"] % 2]
            kp = pr["kp"]
            terms = [(pr["kT"], q_rhs, [pr["k_b"], q_b])] + pr["extra"]
            for ti, (l, r, bs) in enumerate(terms):
                mm(cx, st[:kp, :], l, r, ti == 0, ti == len(terms) - 1, bs, [st_b])
            p, p_b = exp_tile(st, st_b, kp, pr["bias"], pr["bias_b"], cnt["pair"])
            cnt["pair"] += 1
            mm(cx, OACC[0][:, :], pr["v"], p[:kp, :], i == 0, i == n - 1, [pr["v_b"], p_b], [OACC[1]])
            mm(cx, RS[0][:, :], onesb[:kp, :], p[:kp, :], i == 0, i == n - 1, [onesb_b, p_b], [RS[1]])
            kept.append((p, p_b, kp))
        return kept

    def gate_weight(row, Q):
        j = cnt["it"] % 2
        cnt["it"] += 1
        g_ps, g_psb = misc()
        ngq, ngq_b = NGQ[j]
        cx.dma("sp", ngq[:, :], ngT[:, Q * 512:(Q + 1) * 512], ngq_b, [], [ngq_b])
        cx.op("pe", lambda e: e.matmul(g_ps[:, :], gsel[:, row * 128:(row + 1) * 128], ngq[:, :],
                                       start=True, stop=True), [gsel_b, ngq_b], [g_psb])
        cx.op("act", lambda e: e.activation(out=fB[j][0][:, :], in_=g_ps[:, :], func=AF.Sigmoid), [g_psb], [fB[j][1]])
        cx.op("dve", lambda e: e.tensor_scalar(out=fA[j][0][:, :], in0=RS[0][:, :], scalar1=1e-30, scalar2=None,
                                               op0=ALU.max), [RS[1]], [fA[j][1]])
        cx.op("dve", lambda e: e.reciprocal(out=fA[j][0][:, :], in_=fA[j][0][:, :]), [fA[j][1]], [fA[j][1]])
        return j

    def plain_weight():
        j = cnt["it"] % 2
        cnt["it"] += 1
        cx.op("dve", lambda e: e.tensor_scalar(out=fA[j][0][:, :], in0=RS[0][:, :], scalar1=1e-30, scalar2=None,
                                               op0=ALU.max), [RS[1]], [fA[j][1]])
        cx.op("dve", lambda e: e.reciprocal(out=fA[j][0][:, :], in_=fA[j][0][:, :]), [fA[j][1]], [fA[j][1]])
        return j

    kcT_s, kcT_sb = kcc[0]
    vc_s, vc_sb = kcc[1]
    w1s, w1s_b = AT, AT_b
    for which, src in enumerate((kcT, vcT)):
        raw, raw_b = KV[which]
        ld(raw[:, :], raw_b, src)
        ld(w1s[:, 0:4096], w1s_b, w1[which])
        ld(pw2[which][0][:, :], pw2[which][1], w2[which])
        ld(pposT[which][0][:, :], pposT[which][1], posT[which])
        hp, hp_b = misc()
        for r in range(32):
            rhs = raw[:, r:r + 16 * (NCMP - 1) + 1:16]
            mm(cx, hp[:, 0:NCMP], w1s[:, r * 128:(r + 1) * 128], rhs, r == 0, r == 31, [w1s_b, raw_b], [hp_b])
        bp, bp_b = misc()
        for r in range(32):
            mm(cx, bp[:, 0:1], w1s[:, r * 128:(r + 1) * 128], pposT[which][0][:, r:r + 1], r == 0, r == 31,
               [w1s_b, pposT[which][1]], [bp_b])
        cx.op("dve", lambda e: e.tensor_copy(out=cb[:, which:which + 1], in_=bp[:, 0:1]), [bp_b], [cb_b])
        xs, xs_b = fC[0]; x2, x2_b = fC[1]
        cx.op("act", lambda e: e.activation(out=xs[:, 0:NCMP], in_=hp[:, 0:NCMP], func=AF.Identity,
                                            bias=cb[:, which:which + 1], scale=1.0), [hp_b, cb_b], [xs_b])
        cx.op("dve", lambda e: e.tensor_tensor(out=x2[:, 0:NCMP], in0=xs[:, 0:NCMP], in1=xs[:, 0:NCMP], op=ALU.mult),
              [xs_b], [x2_b])
        cx.op("dve", lambda e: e.tensor_scalar(out=x2[:, 0:NCMP], in0=x2[:, 0:NCMP], scalar1=0.044715, scalar2=1.0,
                                               op0=ALU.mult, op1=ALU.add), [x2_b], [x2_b])
        cx.op("dve", lambda e: e.tensor_tensor(out=x2[:, 0:NCMP], in0=x2[:, 0:NCMP], in1=xs[:, 0:NCMP], op=ALU.mult),
              [x2_b, xs_b], [x2_b])
        cx.op("act", lambda e: e.activation(out=x2[:, 0:NCMP], in_=x2[:, 0:NCMP], func=AF.Sigmoid,
                                            scale=2.0 * 0.7978845608028654), [x2_b], [x2_b])
        cx.op("dve", lambda e: e.memset(hid[:, :], 0.0), [], [hid_b])
        cx.op("dve", lambda e: e.tensor_tensor(out=hid[:, 0:NCMP], in0=x2[:, 0:NCMP], in1=xs[:, 0:NCMP], op=ALU.mult),
              [x2_b, xs_b], [hid_b])
        if which == 0:
            op_, op_b = misc()
            mm(cx, op_[:, 0:512], pw2[0][0][:, :], hid[:, :], True, True, [pw2[0][1], hid_b], [op_b])
            cx.op("dve", lambda e: e.tensor_copy(out=kcT_s[:, :], in_=op_[:, 0:512]), [op_b], [kcT_sb])
        else:
            op_, op_b = misc()
            for ntile in range(NCT):
                mm(cx, op_[:, ntile * 128:(ntile + 1) * 128], hid[:, ntile * 128:(ntile + 1) * 128], pw2[1][0][:, :],
                   True, True, [hid_b, pw2[1][1]], [op_b])
            cx.op("dve", lambda e: e.tensor_copy(out=vc_s[:, 0:NCT * 128], in_=op_[:, 0:NCT * 128]), [op_b], [vc_sb])

    ld(EB[0:NSEL, :], EB_b, ensa_d.rearrange("n k p -> n (k p)"))
    AT_written = False
    for Q in range(NQ):
        qs = slice(Q * 512, (Q + 1) * 512)
        nmax = min((512 * Q + 480) // 16, NCMP - 1)
        tiles = list(range(nmax // 128 + 1))
        for h in range(4):
            pairs = []
            for nt_ in tiles:
                kp = min(128, NCMP - nt_ * 128)
                extra = [(onesb[0:2, 0:kp], crw[:, h, :], [onesb_b, crw_b])]
                e_ = Q - 4 * nt_
                if 0 <= e_ <= 4:
                    extra.append((ident[:, 0:kp], masks[:, 8 + e_, :], [ident_b, masks_b]))
                pairs.append(dict(kT=kcT_s[:, nt_ * 128:nt_ * 128 + kp], k_b=kcT_sb, kp=kp, extra=extra,
                                  bias=abc[0:kp, h, nt_ * NQ + Q:nt_ * NQ + Q + 1], bias_b=abc_b,
                                  v=vc_s[0:kp, nt_ * 128:(nt_ + 1) * 128], v_b=vc_sb))
            qt_, qt_b = QT[(Q * 4 + h) % 2]
            ld(qt_[:, :], qt_b, nqT[h][:, qs])
            kept = attn_pairs(qt_[:, :], qt_b, pairs)
            j = gate_weight(h * 3 + 0, Q)
            for ii, (p, p_b, kp) in enumerate(kept):
                cx.op("dve", lambda e: e.tensor_tensor(out=p[:kp, :], in0=p[:kp, :], in1=fA[j][0][:kp, :], op=ALU.mult),
                      [p_b, fA[j][1]], [p_b])
                for ts in range(4):
                    mm(cx, IMP[0][:, ts * 128:ts * 128 + NSEL], p[:kp, ts * 128:(ts + 1) * 128],
                       member[0:kp, tiles[ii], :], h == 0 and ii == 0, h == 3 and ii == len(kept) - 1,
                       [p_b, member_b], [IMP[1]])
            cx.op("dve", lambda e: e.tensor_tensor(out=fA[j][0][:, :], in0=fA[j][0][:, :], in1=fB[j][0][:, :], op=ALU.mult),
                  [fA[j][1], fB[j][1]], [fA[j][1]])
            cx.op("dve", lambda e: e.tensor_tensor(out=fC[j][0][:, :], in0=OACC[0][:, :], in1=fA[j][0][:, :], op=ALU.mult),
                  [OACC[1], fA[j][1]], [fC[j][1]])
            cx.dma("sp", oacc[h * 128:(h + 1) * 128, qs], fC[j][0][:, :], fC[j][1], [fC[j][1]], [oacc_b])
        for ts in range(4):
            tt = Q * 4 + ts
            w0 = 128 - 2 * tt
            if w0 < 0:
                w0 = None
            imp_ap = IMP[0][:, ts * 128:ts * 128 + NSEL]
            c0 = 128 - 2 * tt
            cx.op("dve", lambda e: e.tensor_tensor(out=s1[:, 0:NSEL], in0=imp_ap, in1=selk[:, 0, c0:c0 + NSEL], op=ALU.mult),
                  [IMP[1], selk_b], [s1_b])
            cx.op("dve", lambda e: e.tensor_tensor(out=s1[:, 0:NSEL], in0=s1[:, 0:NSEL], in1=selk[:, 1, c0:c0 + NSEL],
                                                   op=ALU.subtract), [s1_b, selk_b], [s1_b])
            cx.op("dve", lambda e: e.tensor_tensor(out=s1[:, 0:NSEL], in0=s1[:, 0:NSEL], in1=selk[:, 2, c0:c0 + NSEL],
                                                   op=ALU.add), [s1_b, selk_b], [s1_b])
            cx.op("dve", lambda e: e.tensor_scalar(out=s1[:, 0:1], in0=s1[:, 0:1], scalar1=1e4, scalar2=None, op0=ALU.add),
                  [s1_b], [s1_b])
            cx.op("dve", lambda e: e.max(out=m8a[:, :], in_=s1[:, 0:NSEL]), [s1_b], [m8a_b])
            cx.op("dve", lambda e: e.match_replace(out=s2[:, 0:NSEL], in_to_replace=m8a[:, :], in_values=s1[:, 0:NSEL],
                                                   imm_value=-1e30), [m8a_b, s1_b], [s2_b])
            cx.op("dve", lambda e: e.max(out=m8b[:, :], in_=s2[:, 0:NSEL]), [s2_b], [m8b_b])
            cx.op("dve", lambda e: e.tensor_scalar(out=s2[:, 0:NSEL], in0=s1[:, 0:NSEL], scalar1=m8b[:, 7:8], scalar2=None,
                                                   op0=ALU.is_ge), [s1_b, m8b_b], [s2_b])
            cx.op("dve", lambda e: e.tensor_scalar(out=mbt[:, 0:NSEL], in0=s2[:, 0:NSEL], scalar1=-NEG, scalar2=NEG,
                                                   op0=ALU.mult, op1=ALU.add), [s2_b], [mbt_b])
            tp, tp_b = misc()
            mm(cx, tp[0:NSEL, 0:128], mbt[:, 0:NSEL], ident[:, :], True, True, [mbt_b, ident_b], [tp_b])
            cx.op("act", lambda e: e.activation(out=AT[0:NSEL, tt * 128:(tt + 1) * 128], in_=tp[0:NSEL, 0:128], func=AF.Copy),
                  [tp_b], [AT_b])

    ld(KV[0][0][:, :], KV[0][1], ksT)
    ld(KV[1][0][:, :].rearrange("p (k d) -> p k d", d=128), KV[1][1], vs.rearrange("(k p) d -> p k d", p=128))
    ld(KV[2][0][:, :], KV[2][1], kwT)
    ld(KV[3][0][:, :].rearrange("p (k d) -> p k d", d=128), KV[3][1], vw.rearrange("(k p) d -> p k d", p=128))
    for h in range(4):
        ld(QB[h % 2][0][:, :], QB[h % 2][1], nqT[h])
        for Q in range(NQ):
            qs = slice(Q * 512, (Q + 1) * 512)
            q_rhs, q_b = QB[h % 2][0][:, qs], QB[h % 2][1]
            jd = cnt["it"] % 2
            cx.dma("sp", fD[jd][0][:, :], oacc[h * 128:(h + 1) * 128, qs], fD[jd][1], [oacc_b], [fD[jd][1]])
            for br in (1, 2):
                pairs = []
                kts = range(0, 4 * Q + 4) if br == 1 else range(max(0, 4 * Q - 4), 4 * Q + 4)
                kbuf, vbuf = (KV[0], KV[1]) if br == 1 else (KV[2], KV[3])
                for kt in kts:
                    r = 4 * Q - kt
                    extra = [(onesb[0:2, :], crw[:, h, :], [onesb_b, crw_b])]
                    if br == 1:
                        extra.append((EB[0:NSEL, kt * 128:(kt + 1) * 128], AT[0:NSEL, qs], [EB_b, AT_b]))
                    if r <= 0:
                        extra.append((ident[:, :], masks[:, -r, :], [ident_b, masks_b]))
                    elif br == 2:
                        extra.append((ident[:, :], masks[:, 3 + r, :], [ident_b, masks_b]))
                    pairs.append(dict(kT=kbuf[0][:, kt * 128:(kt + 1) * 128], k_b=kbuf[1], kp=128, extra=extra,
                                      bias=ab[:, h, r + 3:r + 4], bias_b=ab_b,
                                      v=vbuf[0][:, kt * 128:(kt + 1) * 128], v_b=vbuf[1]))
                attn_pairs(q_rhs, q_b, pairs)
                j = gate_weight(h * 3 + br, Q)
                cx.op("dve", lambda e: e.tensor_tensor(out=fA[j][0][:, :], in0=fA[j][0][:, :], in1=fB[j][0][:, :],
                                                       op=ALU.mult), [fA[j][1], fB[j][1]], [fA[j][1]])
                cx.op("dve", lambda e: e.tensor_tensor(out=fC[j][0][:, :], in0=OACC[0][:, :], in1=fA[j][0][:, :],
                                                       op=ALU.mult), [OACC[1], fA[j][1]], [fC[j][1]])
                cx.op("dve", lambda e: e.tensor_tensor(out=fD[jd][0][:, :], in0=fD[jd][0][:, :], in1=fC[j][0][:, :],
                                                       op=ALU.add), [fD[jd][1], fC[j][1]], [fD[jd][1]])
            cx.dma("sp", oT[h * 128:(h + 1) * 128, qs], fD[jd][0][:, :], fD[jd][1], [fD[jd][1]], [oT_b])

    ld(EB[0:NB + 2, 0:NB * 128], EB_b, emoba_d.rearrange("n k p -> n (k p)"))
    for h in range(4):
        kb, vb = KV[(h % 2) * 2], KV[(h % 2) * 2 + 1]
        QBh = QB[h % 2]
        ld(QBh[0][:, :], QBh[1], mqT[h])
        ld(kb[0][:, :], kb[1], mkT[h])
        ld(vb[0][:, :].rearrange("p (k d) -> p k d", d=128), vb[1], mv[h].rearrange("(k p) d -> p k d", p=128))
        hh = 4 + h
        for Q in range(NQ):
            cx.dma("pool", AT[NB:NB + 2, Q * 512:(Q + 1) * 512], crow[hh], AT_b, [], [AT_b])
        cx.op("dve", lambda e: e.tensor_reduce(out=kmT[:, 0:NB], in_=kb[0][:, :].rearrange("p (n l) -> p n l", l=256),
                                               axis=AX.X, op=ALU.add), [kb[1]], [kmT_b])
        cx.op("dve", lambda e: e.tensor_scalar(out=kmTb[:, 0:NB], in0=kmT[:, 0:NB], scalar1=1.0 / 256, scalar2=None,
                                               op0=ALU.mult), [kmT_b], [kmTb_b])
        for tt in range(NKT):
            own = tt // 2
            gp, gp_b = misc()
            mm(cx, gp[:, 0:NB], QBh[0][:, tt * 128:(tt + 1) * 128], kmTb[:, 0:NB], True, True, [QBh[1], kmTb_b], [gp_b])
            if GW > NB:
                cx.op("dve", lambda e: e.memset(s1[:, 0:GW], -1e30), [], [s1_b])
            cx.op("dve", lambda e: e.tensor_tensor(out=s1[:, 0:NB], in0=gp[:, 0:NB], in1=U3[:, 2, 32 - own:32 - own + NB],
                                                   op=ALU.add), [gp_b, U3_b], [s1_b])
            cx.op("dve", lambda e: e.max(out=m8a[:, :], in_=s1[:, 0:GW]), [s1_b], [m8a_b])
            cx.op("dve", lambda e: e.tensor_scalar(out=s2[:, 0:NB], in0=s1[:, 0:NB], scalar1=m8a[:, 2:3], scalar2=None,
                                                   op0=ALU.is_ge), [s1_b, m8a_b], [s2_b])
            cx.op("dve", lambda e: e.tensor_tensor(out=s2[:, 0:NB], in0=s2[:, 0:NB], in1=U3[:, 0, 32 - own:32 - own + NB],
                                                   op=ALU.mult), [s2_b, U3_b], [s2_b])
            cx.op("dve", lambda e: e.tensor_tensor(out=s2[:, 0:NB], in0=s2[:, 0:NB], in1=U3[:, 1, 32 - own:32 - own + NB],
                                                   op=ALU.max), [s2_b, U3_b], [s2_b])
            cx.op("dve", lambda e: e.tensor_scalar(out=mbt[:, 0:NB], in0=s2[:, 0:NB], scalar1=-NEG, scalar2=NEG,
                                                   op0=ALU.mult, op1=ALU.add), [s2_b], [mbt_b])
            tp, tp_b = misc()
            mm(cx, tp[0:NB, 0:128], mbt[:, 0:NB], ident[:, :], True, True, [mbt_b, ident_b], [tp_b])
            cx.op("act", lambda e: e.activation(out=AT[0:NB, tt * 128:(tt + 1) * 128], in_=tp[0:NB, 0:128], func=AF.Copy),
                  [tp_b], [AT_b])
        for Q in range(NQ):
            qs = slice(Q * 512, (Q + 1) * 512)
            pairs = []
            for kt in range(0, 4 * Q + 4):
                r = 4 * Q - kt
                blk = kt // 2
                extra = [(EB[0:NB + 2, blk * 128:(blk + 1) * 128], AT[0:NB + 2, qs], [EB_b, AT_b])]
                if r <= 0:
                    extra.append((ident[:, :], masks[:, -r, :], [ident_b, masks_b]))
                pairs.append(dict(kT=kb[0][:, kt * 128:(kt + 1) * 128], k_b=kb[1], kp=128, extra=extra,
                                  bias=ab[:, hh, r + 3:r + 4], bias_b=ab_b,
                                  v=vb[0][:, kt * 128:(kt + 1) * 128], v_b=vb[1]))
            attn_pairs(QBh[0][:, qs], QBh[1], pairs)
            j = plain_weight()
            cx.op("dve", lambda e: e.tensor_tensor(out=fC[j][0][:, :], in0=OACC[0][:, :], in1=fA[j][0][:, :], op=ALU.mult),
                  [OACC[1], fA[j][1]], [fC[j][1]])
            cx.dma("sp", oT[(4 + h) * 128:(5 + h) * 128, qs], fC[j][0][:, :], fC[j][1], [fC[j][1]], [oT_b])
    cx.finish([oT_b])
    return nc


def attn_inputs(nq, kc, vc, ks, vs, kw, vw, ng, mq, mk, mv):
    C = np.ascontiguousarray
    return {"nqT": C(nq.transpose(1, 2, 0)), "kcT": C(kc.T), "vcT": C(vc.T), "ksT": C(ks.T), "vs": C(vs),
            "kwT": C(kw.T), "vw": C(vw), "ngT": C(ng.T), "mqT": C(mq.transpose(1, 2, 0)),
            "mkT": C(mk.transpose(1, 2, 0)), "mv": C(mv.transpose(1, 0, 2))}


def attn_weights(posk, w1k, w2k, posv, w1v, w2v):
    C = np.ascontiguousarray
    t1 = lambda w: C(w.reshape(32, 128, 128).transpose(1, 0, 2)).reshape(128, 32 * 128)
    return {"w1k": t1(w1k), "w1v": t1(w1v), "poskT": C(posk.T), "posvT": C(posv.T), "w2k": C(w2k), "w2v": C(w2v)}


GLA_TAU = 16.0
NORM_EPS = 1e-6


def build_mixer(S):
    nc = bass.Bass("TRN2", target_bir_lowering=False)
    cx = Ctx(nc)
    NT = S // 128
    NQ = S // 512

    def din(name, shape):
        return nc.dram_tensor(name, list(shape), F32, kind="ExternalInput").ap()

    gbT = din("gbT", [512, S]); gcT = din("gcT", [512, S]); hT = din("hT", [512, S]); cw_d = din("cw", [128, 4, 3])
    gqT = din("gqT", [2, 128, S]); gkT = din("gkT", [2, 128, S]); gk = din("gk", [S, 256]); gv = din("gv", [S, 512])
    gg = din("gg", [S, 512]); zaT = din("zaT", [16, S]); wa_d = din("wa", [17, 256]); ngb_d = din("ngb", [128, 512])
    tri_d = din("tri", [128, 2, 128])
    ycT = nc.dram_tensor("ycT", [512, S], F32, kind="ExternalOutput").ap()
    yg = nc.dram_tensor("yg", [S, 512], F32, kind="ExternalOutput").ap()
    ycT_b = cx.buf("ycT"); yg_b = cx.buf("yg")

    def sb(name, shape, dt=F32):
        return nc.alloc_sbuf_tensor("s_" + name, list(shape), dt), cx.buf(name)

    def sb2(name, shape, dt=F32):
        return [sb("%s%d" % (name, i), shape, dt) for i in range(2)]

    PS = [(nc.alloc_psum_tensor("ps%d" % i, [128, 512], F32), cx.buf("ps%d" % i)) for i in range(8)]
    pi = [0]

    def psum():
        pi[0] += 1
        return PS[2 + pi[0] % 6]

    cw, cw_b = sb("cw", [128, 4, 3])
    cx.dma("sp", cw[:, :, :], cw_d, cw_b, [], [cw_b])
    U = sb2("cu", [128, 514]); Hh = sb2("ch", [128, 514]); Gb = sb2("cgb", [128, 512]); Ac = sb2("cacc", [128, 512])
    it = 0
    for Q in range(NQ):
        t0 = Q * 512
        for c in range(4):
            j = it % 2
            it += 1
            u, u_b = U[j]; hh, hh_b = Hh[j]; gb_, gb_b = Gb[j]; ac, ac_b = Ac[j]
            rows = slice(c * 128, (c + 1) * 128)
            if Q == 0:
                cx.op("dve", lambda e: e.memset(u[:, 0:2], 0.0), [], [u_b])
                cx.op("dve", lambda e: e.memset(hh[:, 0:2], 0.0), [], [hh_b])
                cx.dma("sp", u[:, 2:514], gcT[rows, 0:512], u_b, [], [u_b])
                cx.dma("sp", hh[:, 2:514], hT[rows, 0:512], hh_b, [], [hh_b])
            else:
                cx.dma("sp", u[:, :], gcT[rows, t0 - 2:t0 + 512], u_b, [], [u_b])
                cx.dma("sp", hh[:, :], hT[rows, t0 - 2:t0 + 512], hh_b, [], [hh_b])
            cx.dma("sp", gb_[:, :], gbT[rows, t0:t0 + 512], gb_b, [], [gb_b])
            cx.op("dve", lambda e: e.tensor_tensor(out=u[:, :], in0=u[:, :], in1=hh[:, :], op=ALU.mult), [u_b, hh_b], [u_b])
            cx.op("dve", lambda e: e.tensor_scalar(out=ac[:, :], in0=u[:, 2:514], scalar1=cw[:, c, 2:3], scalar2=None,
                                                   op0=ALU.mult), [u_b, cw_b], [ac_b])
            cx.op("dve", lambda e: e.scalar_tensor_tensor(out=ac[:, :], in0=u[:, 1:513], scalar=cw[:, c, 1:2], in1=ac[:, :],
                                                          op0=ALU.mult, op1=ALU.add), [u_b, cw_b, ac_b], [ac_b])
            cx.op("dve", lambda e: e.scalar_tensor_tensor(out=ac[:, :], in0=u[:, 0:512], scalar=cw[:, c, 0:1], in1=ac[:, :],
                                                          op0=ALU.mult, op1=ALU.add), [u_b, cw_b, ac_b], [ac_b])
            cx.op("dve", lambda e: e.tensor_tensor(out=ac[:, :], in0=ac[:, :], in1=gb_[:, :], op=ALU.mult), [ac_b, gb_b], [ac_b])
            cx.dma("sp", ycT[rows, t0:t0 + 512], ac[:, :], ac_b, [ac_b], [ycT_b])

    wa, wa_b = sb("wa", [17, 256]); ngb, ngb_b = sb("ngb", [128, 512]); tri, tri_b = sb("tri", [128, 2, 128])
    trib, trib_b = sb("trib", [128, 128])
    cx.dma("sp", wa[:, :], wa_d, wa_b, [], [wa_b])
    cx.dma("sp", ngb[:, :], ngb_d, ngb_b, [], [ngb_b])
    cx.dma("sp", tri[:, :, :], tri_d, tri_b, [], [tri_b])
    Sst = [sb("S%d" % i, [128, 256]) for i in range(2)]
    Sbf = [sb("Sb%d" % i, [128, 256], BF16) for i in range(2)]
    for i in range(2):
        cx.op("dve", lambda e: e.memset(Sst[i][0][:, :], 0.0), [], [Sst[i][1]])
        cx.op("dve", lambda e: e.memset(Sbf[i][0][:, :], 0.0), [], [Sbf[i][1]])
    ZA = sb2("za", [17, 128]); SP = sb2("sp", [128, 256]); KD = sb2("kd", [128, 256], BF16); KDEC = sb2("kdec", [128, 256])
    KTM = sb2("ktm", [128, 256]); VF = sb2("vf", [128, 512]); VB = sb2("vb", [128, 512], BF16); GG = sb2("gg", [128, 512])
    QF = [sb2("qf%d" % p, [128, 128]) for p in range(2)]; KF = [sb2("kf%d" % p, [128, 128]) for p in range(2)]
    EQ = [sb2("eq%d" % p, [128, 128]) for p in range(2)]; EK = [sb2("ek%d" % p, [128, 128]) for p in range(2)]
    QT = [sb2("qtb%d" % p, [128, 128], BF16) for p in range(2)]; KT = [sb2("ktb%d" % p, [128, 128], BF16) for p in range(2)]
    DEC = [sb2("dec%d" % p, [128, 1]) for p in range(2)]
    ATM = [sb("atm%d" % i, [128, 128], BF16) for i in range(4)]
    OSQ = sb2("osq", [128, 512]); SS = sb2("ss", [128, 4]); Y = sb2("y", [128, 512])
    for j in range(2):
        cx.op("dve", lambda e: e.memset(ZA[j][0][0:1, :], 1.0), [], [ZA[j][1]])
    for t in range(NT):
        j = t % 2
        ts = slice(t * 128, (t + 1) * 128)
        za, za_b = ZA[j]
        cx.dma("sp", za[1:17, :], zaT[:, ts], za_b, [], [za_b])
        ktm, ktm_b = KTM[j]; vf, vf_b = VF[j]; vb, vb_b = VB[j]; ggt, gg_b = GG[j]
        cx.dma("sp", ktm[:, :], gk[ts, :], ktm_b, [], [ktm_b])
        cx.dma("sp", vf[:, :], gv[ts, :], vf_b, [], [vf_b])
        cx.dma("sp", ggt[:, :], gg[ts, :], gg_b, [], [gg_b])
        cx.op("act", lambda e: e.activation(out=vb[:, :], in_=vf[:, :], func=AF.Copy), [vf_b], [vb_b])
        zp, zp_b = psum()
        mm(cx, zp[:, 0:256], za[:, :], wa[:, :], True, True, [za_b, wa_b], [zp_b])
        sp_, sp_b = SP[j]
        cx.op("act", lambda e: e.activation(out=sp_[:, :], in_=zp[:, 0:256], func=AF.Exp, scale=-1.0), [zp_b], [sp_b])
        cx.op("act", lambda e: e.activation(out=sp_[:, :], in_=sp_[:, :], func=AF.Ln, bias=1.0, scale=1.0), [sp_b], [sp_b])
        dp, dp_b = psum()
        mm(cx, dp[:, 0:256], tri[:, 1, :], sp_[:, :], True, True, [tri_b, sp_b], [dp_b])
        kdec, kdec_b = KDEC[j]; kd, kd_b = KD[j]
        cx.op("act", lambda e: e.activation(out=kdec[:, :], in_=dp[:, 0:256], func=AF.Exp, scale=-1.0 / GLA_TAU),
              [dp_b], [kdec_b])
        cx.op("dve", lambda e: e.tensor_tensor(out=kd[:, :], in0=ktm[:, :], in1=kdec[:, :], op=ALU.mult),
              [ktm_b, kdec_b], [kd_b])
        op_, op_b = PS[t % 2]
        for pr in range(2):
            qf, qf_b = QF[pr][j]; kf, kf_b = KF[pr][j]
            cx.dma("sp", qf[:, :], gqT[pr][:, ts], qf_b, [], [qf_b])
            cx.dma("sp", kf[:, :], gkT[pr][:, ts], kf_b, [], [kf_b])
            bp, bp_b = psum()
            mm(cx, bp[:, 0:128], sp_[:, pr * 128:(pr + 1) * 128], tri[:, 0, :], True, True, [sp_b, tri_b], [bp_b])
            eq, eq_b = EQ[pr][j]; ek, ek_b = EK[pr][j]; qt, qt_b = QT[pr][j]; kt, kt_b = KT[pr][j]; dec, dec_b = DEC[pr][j]
            cx.op("act", lambda e: e.activation(out=eq[:, :], in_=bp[:, 0:128], func=AF.Exp, scale=-1.0 / GLA_TAU,
                                                bias=math.log(0.125)), [bp_b], [eq_b])
            cx.op("act", lambda e: e.activation(out=ek[:, :], in_=bp[:, 0:128], func=AF.Exp, scale=1.0 / GLA_TAU),
                  [bp_b], [ek_b])
            cx.op("act", lambda e: e.activation(out=dec[:, :], in_=bp[:, 127:128], func=AF.Exp, scale=-1.0 / GLA_TAU),
                  [bp_b], [dec_b])
            cx.op("dve", lambda e: e.tensor_tensor(out=qt[:, :], in0=qf[:, :], in1=eq[:, :], op=ALU.mult), [qf_b, eq_b], [qt_b])
            cx.op("dve", lambda e: e.tensor_tensor(out=kt[:, :], in0=kf[:, :], in1=ek[:, :], op=ALU.mult), [kf_b, ek_b], [kt_b])
            for hf in range(2):
                h = pr * 2 + hf
                prt = slice(hf * 64, (hf + 1) * 64)
                ap_, ap_b = psum()
                mm(cx, ap_[:, 0:128], kt[prt, :], qt[prt, :], True, True, [kt_b, qt_b], [ap_b])
                atm, atm_b = ATM[h]
                cx.op("dve", lambda e: e.tensor_tensor(out=atm[:, :], in0=ap_[:, 0:128], in1=tri[:, 0, :], op=ALU.mult),
                      [ap_b, tri_b], [atm_b])
                mm(cx, op_[:, h * 128:(h + 1) * 128], atm[:, :], vb[:, h * 128:(h + 1) * 128], True, False,
                   [atm_b, vb_b], [op_b])
                mm(cx, op_[:, h * 128:(h + 1) * 128], qt[prt, :], Sbf[pr][0][prt, hf * 128:(hf + 1) * 128], False, True,
                   [qt_b, Sbf[pr][1]], [op_b])
            up, up_b = psum()
            mm(cx, up[:, 0:256], kd[:, pr * 128:(pr + 1) * 128], vb[:, pr * 256:(pr + 1) * 256], True, True,
               [kd_b, vb_b], [up_b])
            cx.op("dve", lambda e: e.scalar_tensor_tensor(out=Sst[pr][0][:, :], in0=Sst[pr][0][:, :], scalar=dec[:, 0:1],
                                                          in1=up[:, 0:256], op0=ALU.mult, op1=ALU.add),
                  [Sst[pr][1], dec_b, up_b], [Sst[pr][1]])
            cx.op("act", lambda e: e.activation(out=Sbf[pr][0][:, :], in_=Sst[pr][0][:, :], func=AF.Copy),
                  [Sst[pr][1]], [Sbf[pr][1]])
        osq, osq_b = OSQ[j]; ss, ss_b = SS[j]; y, y_b = Y[j]
        cx.op("act", lambda e: e.activation(out=osq[:, :], in_=op_[:, :], func=AF.Square), [op_b], [osq_b])
        cx.op("dve", lambda e: e.tensor_reduce(out=ss[:, :], in_=osq[:, :].rearrange("p (h d) -> p h d", d=128), axis=AX.X,
                                               op=ALU.add), [osq_b], [ss_b])
        cx.op("dve", lambda e: e.tensor_scalar(out=ss[:, :], in0=ss[:, :], scalar1=1.0 / 128, scalar2=NORM_EPS,
                                               op0=ALU.mult, op1=ALU.add), [ss_b], [ss_b])
        cx.op("act", lambda e: e.activation(out=ss[:, :], in_=ss[:, :], func=AF.Sqrt), [ss_b], [ss_b])
        cx.op("dve", lambda e: e.reciprocal(out=ss[:, :], in_=ss[:, :]), [ss_b], [ss_b])
        for h in range(4):
            cx.op("dve", lambda e: e.tensor_scalar(out=y[:, h * 128:(h + 1) * 128], in0=op_[:, h * 128:(h + 1) * 128],
                                                   scalar1=ss[:, h:h + 1], scalar2=None, op0=ALU.mult),
                  [op_b, ss_b], [y_b])
        cx.op("dve", lambda e: e.tensor_tensor(out=y[:, :], in0=y[:, :], in1=ngb[:, :], op=ALU.mult), [y_b, ngb_b], [y_b])
        cx.op("act", lambda e: e.activation(out=ggt[:, :], in_=ggt[:, :], func=AF.Silu), [gg_b], [gg_b])
        cx.op("dve", lambda e: e.tensor_tensor(out=y[:, :], in0=y[:, :], in1=ggt[:, :], op=ALU.mult), [y_b, gg_b], [y_b])
        cx.dma("sp", yg[ts, :], y[:, :], y_b, [y_b], [yg_b])
    cx.finish([ycT_b, yg_b])
    return nc


def mixer_inputs(gb, gc, hh, q, k, v, g, za, conv_w, w_a2, b_a, norm_g):
    C = np.ascontiguousarray
    j = np.arange(128)
    tri = np.stack([(j[:, None] <= j[None, :]), (j[:, None] > j[None, :])], axis=1).astype(np.float32)
    return {"gbT": C(gb.T), "gcT": C(gc.T), "hT": C(hh.T), "cw": C(conv_w.T.reshape(4, 128, 3).transpose(1, 0, 2)),
            "gqT": C(q.T.reshape(2, 128, -1)), "gkT": C(k.T.reshape(2, 128, -1)), "gk": C(k), "gv": C(v), "gg": C(g),
            "zaT": C(za.T), "wa": C(np.concatenate([b_a[None, :], w_a2], axis=0)),
            "ngb": C(np.tile(norm_g[None, :], (128, 4))), "tri": tri}


_PROGS = {}


def _prog(key, builder):
    if key not in _PROGS:
        _PROGS[key] = builder()
    return _PROGS[key]


def _run(nc, in_maps):
    res = run_bass_kernel_spmd(nc, in_maps, core_ids=list(range(NCORES)))
    return res.results


T_CORE = BATCH * SEQ // NCORES
L0_SPL = (2048, 512, 512, 512, 512, 512, 512, 48, 2048, 2048, 2048)
L1_SPL = (2048, 2048, 2048, 1024, 1024, 2048, 2048, 16)


def _tok_shards_T(a):
    return [np.ascontiguousarray(a[c * T_CORE:(c + 1) * T_CORE].T) for c in range(NCORES)]


def _gather_T(results, key="yT", ncols=None):
    out = np.concatenate([r[key].T for r in results], axis=0)
    return out if ncols is None else out[:, :ncols]


def _ffn(xf, wg, wu, wd, g, b):
    nc = _prog("ffn", lambda: build_dense("ffn", D_MODEL, D_FF, T_CORE, 512))
    shared = {"wg": tile_w(wg), "wu": tile_w(wu), "wd": tile_w(wd), "lng": ln_tile(g), "lnb": ln_tile(b)}
    xs = _tok_shards_T(xf)
    return _gather_T(_run(nc, [dict(shared, xT=xs[c]) for c in range(NCORES)]))


def _lin(xf, w):
    n = w.shape[1]
    npad = ((n + 127) // 128) * 128
    nc = _prog("lin%d" % npad, lambda: build_dense("lin", D_MODEL, 0, T_CORE, 512, NOUT=npad))
    shared = {"wl": tile_w(w, npad)}
    xs = _tok_shards_T(xf)
    return _gather_T(_run(nc, [dict(shared, xT=xs[c]) for c in range(NCORES)]), ncols=n)


def _outp(xf, of, w, g, b):
    nc = _prog("out", lambda: build_dense("out", D_MODEL, 0, T_CORE, 512))
    shared = {"wd": tile_w(w), "lng": ln_tile(g), "lnb": ln_tile(b)}
    xs = _tok_shards_T(xf)
    os_ = _tok_shards_T(of)
    return _gather_T(_run(nc, [dict(shared, xT=xs[c], oT=os_[c]) for c in range(NCORES)]))


def _split(z, sizes):
    idx = np.cumsum(sizes)[:-1]
    return np.split(z, idx, axis=-1)


def _attn_core(z, pos_k, w1_k, w2_k, pos_v, w1_v, w2_v):
    nc = _prog("attn", lambda: build_attn(SEQ))
    nq, kc, vc, ks, vs, kw, vw, ng, mq, mk, mv = _split(z, L0_SPL)
    slopes = 2.0 ** (-8.0 * np.arange(1, 33, dtype=np.float64) / 32)
    sn = slopes[0::2].reshape(4, 4)
    sm = slopes[1::2]
    wts = attn_weights(pos_k, w1_k, w2_k, pos_v, w1_v, w2_v)
    in_maps = []
    for c in range(NCORES):
        b, g = divmod(c, 4)
        r = slice(b * SEQ, (b + 1) * SEQ)
        s5, s1 = slice(g * 512, (g + 1) * 512), slice(g * 128, (g + 1) * 128)
        im = attn_inputs(nq[r, s5].reshape(SEQ, 4, 128), kc[r, s1], vc[r, s1], ks[r, s1], vs[r, s1], kw[r, s1], vw[r, s1],
                         ng[r, g * 12:(g + 1) * 12], mq[r, s5].reshape(SEQ, 4, 128), mk[r, s5].reshape(SEQ, 4, 128),
                         mv[r, s5].reshape(SEQ, 4, 128))
        im.update(wts)
        im.update(attn_consts(SEQ, sn[g], sm[4 * g:4 * g + 4]))
        in_maps.append(im)
    res = _run(nc, in_maps)
    o = np.empty((BATCH * SEQ, 4096), np.float32)
    for c in range(NCORES):
        b, g = divmod(c, 4)
        oT = res[c]["oT"]
        o[b * SEQ:(b + 1) * SEQ, g * 512:(g + 1) * 512] = oT[0:512].T
        o[b * SEQ:(b + 1) * SEQ, 2048 + g * 512:2048 + (g + 1) * 512] = oT[512:1024].T
    return o


def _mixer_core(z, conv_w, w_a2, b_a, norm_g):
    nc = _prog("mixer", lambda: build_mixer(SEQ))
    gb, gc, hh, q, k, v, g_, za = _split(z, L1_SPL)
    in_maps = []
    for c in range(NCORES):
        b, pt = divmod(c, 4)
        r = slice(b * SEQ, (b + 1) * SEQ)
        s5, s2 = slice(pt * 512, (pt + 1) * 512), slice(pt * 256, (pt + 1) * 256)
        in_maps.append(mixer_inputs(gb[r, s5], gc[r, s5], hh[r, s5], q[r, s2], k[r, s2], v[r, s5], g_[r, s5], za[r],
                                    conv_w[:, s5], w_a2[:, s2], b_a[s2], norm_g))
    res = _run(nc, in_maps)
    m = np.empty((BATCH * SEQ, 4096), np.float32)
    for c in range(NCORES):
        b, pt = divmod(c, 4)
        m[b * SEQ:(b + 1) * SEQ, pt * 512:(pt + 1) * 512] = res[c]["ycT"].T
        m[b * SEQ:(b + 1) * SEQ, 2048 + pt * 512:2048 + (pt + 1) * 512] = res[c]["yg"]
    return m


def kernel(x, ln_g, ln_b, ffn_pre_wg, ffn_pre_wu, ffn_pre_wd, ffn_post_wg, ffn_post_wu, ffn_post_wd,
           att_w_in, att_w_out, nsa_pos_k, nsa_w1_k, nsa_w2_k, nsa_pos_v, nsa_w1_v, nsa_w2_v,
           mix_w_in, mix_w_out, conv_w, gla_w_a2, gla_b_a, gla_norm_g):
    f = lambda a: np.asarray(a, dtype=np.float32)
    xf = f(x).reshape(BATCH * SEQ, D_MODEL)
    ln_g, ln_b = f(ln_g), f(ln_b)
    xf = _ffn(xf, f(ffn_pre_wg)[0], f(ffn_pre_wu)[0], f(ffn_pre_wd)[0], ln_g[0, 0], ln_b[0, 0])
    z = _lin(xf, f(att_w_in)[0])
    o = _attn_core(z, f(nsa_pos_k)[0], f(nsa_w1_k)[0], f(nsa_w2_k)[0], f(nsa_pos_v)[0], f(nsa_w1_v)[0], f(nsa_w2_v)[0])
    del z
    xf = _outp(xf, o, f(att_w_out)[0], ln_g[0, 1], ln_b[0, 1])
    xf = _ffn(xf, f(ffn_post_wg)[0], f(ffn_post_wu)[0], f(ffn_post_wd)[0], ln_g[0, 2], ln_b[0, 2])
    xf = _ffn(xf, f(ffn_pre_wg)[1], f(ffn_pre_wu)[1], f(ffn_pre_wd)[1], ln_g[1, 0], ln_b[1, 0])
    z = _lin(xf, f(mix_w_in)[0])
    m = _mixer_core(z, f(conv_w)[0], f(gla_w_a2)[0], f(gla_b_a)[0], f(gla_norm_g)[0])
    del z
    xf = _outp(xf, m, f(mix_w_out)[0], ln_g[1, 1], ln_b[1, 1])
    xf = _ffn(xf, f(ffn_post_wg)[1], f(ffn_post_wu)[1], f(ffn_post_wd)[1], ln_g[1, 2], ln_b[1, 2])
    return xf.reshape(BATCH, SEQ, D_MODEL)
```

```python
import math
import numpy as np
import concourse.bass as bass
import concourse.mybir as mybir
from concourse.bass_utils import run_bass_kernel_spmd

F32 = mybir.dt.float32
BF16 = mybir.dt.bfloat16
AF = mybir.ActivationFunctionType
ALU = mybir.AluOpType
AX = mybir.AxisListType

D_MODEL = 4096
BATCH = 2
SEQ = 8192
DEPTH = 2
D_FF = 11008
ALPHA = (2 * DEPTH) ** 0.25
LN_EPS = 1e-5
NCORES = 8


class Buf:
    __slots__ = ("name", "w", "rs", "dsem", "dcnt")

    def __init__(self, name):
        self.name = name
        self.w = None
        self.rs = {}
        self.dsem = None
        self.dcnt = 0


class Ctx:
    def __init__(self, nc):
        self.nc = nc
        self.E = {"pe": nc.tensor, "act": nc.scalar, "dve": nc.vector, "pool": nc.gpsimd, "sp": nc.sync}
        self.esem = {e: nc.alloc_semaphore("es_" + e) for e in self.E}
        self.ecnt = {e: 0 for e in self.E}
        self.seen = {e: {} for e in self.E}
        self.semcnt = {}
        self.nbuf = 0

    def buf(self, name=None):
        self.nbuf += 1
        return Buf(name or "b%d" % self.nbuf)

    def bufs(self, n, name="b"):
        return [self.buf("%s%d" % (name, i)) for i in range(n)]

    def _wait(self, e, tok):
        sem, val, own = tok
        if own == e and e in ("pe", "sp"):
            return
        if own == e and val > self.ecnt[e]:
            return
        if sem.num in self.semcnt:
            val = max(val, self.semcnt[sem.num])
        if self.seen[e].get(sem.num, 0) >= val:
            return
        self.E[e].wait_ge(sem, val)
        self.seen[e][sem.num] = val

    def _deps(self, e, reads, writes):
        for b in reads:
            if b.w is not None:
                self._wait(e, b.w)
        for b in writes:
            if b.w is not None:
                self._wait(e, b.w)
            for t in b.rs.values():
                self._wait(e, t)

    def _record(self, tok, reads, writes):
        for b in reads:
            b.rs[tok[0].num] = tok
        for b in writes:
            b.w = tok
            b.rs = {}

    def op(self, e, fn, reads=(), writes=(), inc=True):
        self._deps(e, reads, writes)
        ins = fn(self.E[e])
        if inc:
            self.ecnt[e] += 1
            ins.then_inc(self.esem[e], 1)
            tok = (self.esem[e], self.ecnt[e], e)
        else:
            tok = (self.esem[e], self.ecnt[e] + 1, e)
        self._record(tok, reads, writes)
        return ins

    def dma(self, q, out, in_, sb, reads=(), writes=()):
        self._deps(q, reads, writes)
        if sb.dsem is None:
            sb.dsem = self.nc.alloc_semaphore("ds_" + sb.name)
        ins = self.E[q].dma_start(out=out, in_=in_)
        sb.dcnt += 16
        ins.then_inc(sb.dsem, 16)
        self.semcnt[sb.dsem.num] = sb.dcnt
        tok = (sb.dsem, sb.dcnt, "dma")
        self._record(tok, reads, writes)
        return ins

    def finish(self, bufs):
        for b in bufs:
            if b.w is not None:
                self._wait("sp", b.w)


def mm(cx, out_ap, lhsT, rhs, start, stop, reads, writes):
    cx.op("pe", lambda e: e.matmul(out_ap, lhsT, rhs, start=start, stop=stop), reads, writes, inc=stop)


def build_dense(mode, D, F, T, TG, NOUT=None):
    nc = bass.Bass("TRN2", target_bir_lowering=False)
    cx = Ctx(nc)
    KD = D // 128
    NG = T // TG
    dram = {}

    def din(name, shape, dt=F32):
        dram[name] = nc.dram_tensor(name, list(shape), dt, kind="ExternalInput").ap()
        return dram[name]

    xT = din("xT", [D, T])
    if mode == "ffn":
        KF = F // 128
        wg = din("wg", [KF, 128, KD * 128])
        wu = din("wu", [KF, 128, KD * 128])
        wd = din("wd", [KD, 128, KF * 128])
        KB = KF
    elif mode == "out":
        oT = din("oT", [D, T])
        wd = din("wd", [KD, 128, KD * 128])
        KB = KD
    else:
        NT = NOUT // 128
        wl = din("wl", [NT, 128, KD * 128])
    if mode != "lin":
        lng = din("lng", [128, KD])
        lnb = din("lnb", [128, KD])
        yT = nc.dram_tensor("yT", [D, T], F32, kind="ExternalOutput").ap()
        vscr = nc.dram_tensor("vscr", [D, T], F32, kind="Internal").ap()
        res_c = (0.5 / ALPHA) if mode == "ffn" else (1.0 / ALPHA)
        eps = LN_EPS / (ALPHA * ALPHA)
    else:
        yT = nc.dram_tensor("yT", [NOUT, T], F32, kind="ExternalOutput").ap()
    yT_b = cx.buf("yT")
    vscr_b = cx.buf("vscr")
    wscr = {}

    def scr(name, n, width):
        wscr[name] = (nc.dram_tensor("scr_" + name, [n, 128, width], BF16, kind="Internal").ap(), cx.bufs(n, "scr_" + name))

    def wload(name, src, idx, s_, lo, width, g):
        if NG == 1:
            cx.dma("pool", wsl[s_][:, lo:lo + width], src[idx], wsl_b[s_], [], [wsl_b[s_]])
            return
        d_ap, d_b = wscr[name]
        if g == 0:
            cx.dma("pool", wsl[s_][:, lo:lo + width], src[idx], wsl_b[s_], [], [wsl_b[s_]])
        else:
            cx.dma("pool", wsl[s_][:, lo:lo + width], d_ap[idx], wsl_b[s_], [d_b[idx]], [wsl_b[s_]])

    def wsave(name, idx, s_, lo, width, g):
        if NG > 1 and g == 0:
            d_ap, d_b = wscr[name]
            cx.dma("sp", d_ap[idx], wsl[s_][:, lo:lo + width], wsl_b[s_], [wsl_b[s_]], [d_b[idx]])

    xb = nc.alloc_sbuf_tensor("xb", [128, KD, TG], BF16)
    xb_b = cx.buf("xb")
    if mode == "lin":
        WSZ = KD * 128
    else:
        WSZ = max(KB * 128, 2 * KD * 128 if mode == "ffn" else 0)
    wsl = [nc.alloc_sbuf_tensor("w%d" % i, [128, WSZ], BF16) for i in range(2)]
    wsl_b = [cx.buf("w%d" % i) for i in range(2)]
    if mode != "lin":
        hT = nc.alloc_sbuf_tensor("hT", [128, KB, TG], BF16)
        hT_b = cx.bufs(KB, "hT")
        ones = nc.alloc_sbuf_tensor("ones", [128, 128], F32)
        ones_b = cx.buf("ones")
        lng_s = nc.alloc_sbuf_tensor("lng_s", [128, KD], F32)
        lnb_s = nc.alloc_sbuf_tensor("lnb_s", [128, KD], F32)
        ln_b = cx.buf("ln")
        cx.op("dve", lambda e: e.memset(ones[:, :], 1.0), [], [ones_b])
        cx.dma("sp", lng_s[:, :], lng, ln_b, [], [ln_b])
        cx.dma("sp", lnb_s[:, :], lnb, ln_b, [], [ln_b])

    def tiles(name, n, dt=F32, w=None):
        w = w or TG
        return ([nc.alloc_sbuf_tensor("%s%d" % (name, i), [128, w], dt) for i in range(n)], cx.bufs(n, name))

    sg, sg_b = tiles("sg", 2)
    if mode != "lin":
        xr, xr_b = tiles("xr", 2)
        vt, vt_b = tiles("vt", 2)
        sq, sq_b = tiles("sq", 2)
        t1, t1_b = tiles("t1", 2)
        yt, yt_b = tiles("yt", 2)
        st, st_b = tiles("st", 4)
    ps = [nc.alloc_psum_tensor("ps%d" % i, [128, 512], F32) for i in range(8)]
    ps_b = cx.bufs(8, "ps")
    if mode == "ffn":
        scr("wg", KF, KD * 128); scr("wu", KF, KD * 128); scr("wd", KD, KF * 128)
    elif mode == "out":
        scr("wd", KD, KB * 128)
    else:
        scr("wl", NT, KD * 128)

    xT_v = xT.rearrange("(k p) t -> p k t", p=128)
    if mode == "out":
        oT_v = oT.rearrange("(k p) t -> p k t", p=128)
    wcount = [0]

    def wslot():
        s = wcount[0] % 2
        wcount[0] += 1
        return s

    for g in range(NG):
        tsl = slice(g * TG, (g + 1) * TG)
        if mode in ("ffn", "lin"):
            cx.dma("pool", xb[:, :, :], xT_v[:, :, tsl], xb_b, [], [xb_b])
        if mode == "ffn":
            for ft in range(KF):
                s = wslot()
                wload("wg", wg, ft, s, 0, KD * 128, g)
                wload("wu", wu, ft, s, KD * 128, KD * 128, g)
                pa, pb = ft % 2, 2 + ft % 2
                for k in range(KD):
                    mm(cx, ps[pa][:, 0:TG], wsl[s][:, k * 128:(k + 1) * 128], xb[:, k, :], k == 0, k == KD - 1,
                       [wsl_b[s], xb_b], [ps_b[pa]])
                for k in range(KD):
                    mm(cx, ps[pb][:, 0:TG], wsl[s][:, (KD + k) * 128:(KD + k + 1) * 128], xb[:, k, :], k == 0,
                       k == KD - 1, [wsl_b[s], xb_b], [ps_b[pb]])
                wsave("wg", ft, s, 0, KD * 128, g)
                wsave("wu", ft, s, KD * 128, KD * 128, g)
                j = ft % 2
                cx.op("act", lambda e: e.activation(out=sg[j][:, :], in_=ps[pa][:, 0:TG], func=AF.Silu),
                      [ps_b[pa]], [sg_b[j]])
                cx.op("dve", lambda e: e.tensor_tensor(out=hT[:, ft, :], in0=sg[j][:, :], in1=ps[pb][:, 0:TG],
                                                       op=ALU.mult), [sg_b[j], ps_b[pb]], [hT_b[ft]])
        elif mode == "lin":
            for nt in range(NT):
                s = wslot()
                wload("wl", wl, nt, s, 0, KD * 128, g)
                pa = nt % 2
                for k in range(KD):
                    mm(cx, ps[pa][:, 0:TG], wsl[s][:, k * 128:(k + 1) * 128], xb[:, k, :], k == 0, k == KD - 1,
                       [wsl_b[s], xb_b], [ps_b[pa]])
                wsave("wl", nt, s, 0, KD * 128, g)
                j = nt % 2
                cx.op("act", lambda e: e.activation(out=sg[j][:, :], in_=ps[pa][:, 0:TG], func=AF.Copy),
                      [ps_b[pa]], [sg_b[j]])
                cx.dma("sp", yT[nt * 128:(nt + 1) * 128, tsl], sg[j][:, :], sg_b[j], [sg_b[j]], [yT_b])
            continue
        else:
            cx.dma("pool", hT[:, :, :], oT_v[:, :, tsl], hT_b[0], [], hT_b)
        for dt_ in range(KD):
            s = wslot()
            wload("wd", wd, dt_, s, 0, KB * 128, g)
            j = dt_ % 2
            cx.dma("sp", xr[j][:, :], xT[dt_ * 128:(dt_ + 1) * 128, tsl], xr_b[j], [], [xr_b[j]])
            po = 4 + j
            for k in range(KB):
                mm(cx, ps[po][:, 0:TG], wsl[s][:, k * 128:(k + 1) * 128], hT[:, k, :], k == 0, k == KB - 1,
                   [wsl_b[s], hT_b[k]], [ps_b[po]])
            wsave("wd", dt_, s, 0, KB * 128, g)
            cx.op("dve", lambda e: e.scalar_tensor_tensor(out=vt[j][:, :], in0=ps[po][:, 0:TG], scalar=res_c,
                                                          in1=xr[j][:, :], op0=ALU.mult, op1=ALU.add),
                  [ps_b[po], xr_b[j]], [vt_b[j]])
            cx.op("act", lambda e: e.activation(out=sq[j][:, :], in_=vt[j][:, :], func=AF.Square),
                  [vt_b[j]], [sq_b[j]])
            mm(cx, ps[6][:, 0:TG], ones[:, :], vt[j][:, :], dt_ == 0, dt_ == KD - 1, [ones_b, vt_b[j]], [ps_b[6]])
            mm(cx, ps[7][:, 0:TG], ones[:, :], sq[j][:, :], dt_ == 0, dt_ == KD - 1, [ones_b, sq_b[j]], [ps_b[7]])
            cx.dma("sp", vscr[dt_ * 128:(dt_ + 1) * 128, tsl], vt[j][:, :], vt_b[j], [vt_b[j]], [vscr_b])
        mean, var, rstd, nmr = st
        mean_b, var_b, rstd_b, nmr_b = st_b
        cx.op("dve", lambda e: e.tensor_scalar(out=mean[:, :], in0=ps[6][:, 0:TG], scalar1=1.0 / D, scalar2=None,
                                               op0=ALU.mult), [ps_b[6]], [mean_b])
        cx.op("dve", lambda e: e.tensor_tensor(out=var[:, :], in0=mean[:, :], in1=mean[:, :], op=ALU.mult),
              [mean_b], [var_b])
        cx.op("dve", lambda e: e.scalar_tensor_tensor(out=var[:, :], in0=ps[7][:, 0:TG], scalar=1.0 / D,
                                                      in1=var[:, :], op0=ALU.mult, op1=ALU.subtract),
              [ps_b[7], var_b], [var_b])
        cx.op("dve", lambda e: e.tensor_scalar(out=var[:, :], in0=var[:, :], scalar1=eps, scalar2=None,
                                               op0=ALU.add), [var_b], [var_b])
        cx.op("act", lambda e: e.activation(out=var[:, :], in_=var[:, :], func=AF.Sqrt), [var_b], [var_b])
        cx.op("dve", lambda e: e.reciprocal(out=rstd[:, :], in_=var[:, :]), [var_b], [rstd_b])
        cx.op("dve", lambda e: e.scalar_tensor_tensor(out=nmr[:, :], in0=mean[:, :], scalar=-1.0, in1=rstd[:, :],
                                                      op0=ALU.mult, op1=ALU.mult), [mean_b, rstd_b], [nmr_b])
        for dt_ in range(KD):
            j = dt_ % 2
            cx.dma("sp", vt[j][:, :], vscr[dt_ * 128:(dt_ + 1) * 128, tsl], vt_b[j], [vscr_b], [vt_b[j]])
            cx.op("dve", lambda e: e.tensor_tensor(out=t1[j][:, :], in0=vt[j][:, :], in1=rstd[:, :], op=ALU.mult),
                  [vt_b[j], rstd_b], [t1_b[j]])
            cx.op("dve", lambda e: e.tensor_tensor(out=t1[j][:, :], in0=t1[j][:, :], in1=nmr[:, :], op=ALU.add),
                  [t1_b[j], nmr_b], [t1_b[j]])
            cx.op("act", lambda e: e.activation(out=yt[j][:, :], in_=t1[j][:, :], func=AF.Identity,
                                                bias=lnb_s[:, dt_:dt_ + 1], scale=lng_s[:, dt_:dt_ + 1]),
                  [t1_b[j], ln_b], [yt_b[j]])
            cx.dma("sp", yT[dt_ * 128:(dt_ + 1) * 128, tsl], yt[j][:, :], yt_b[j], [yt_b[j]], [yT_b])
    cx.finish([yT_b])
    return nc


def tile_w(w, ncols_pad=None):
    K, N = w.shape
    if ncols_pad is not None and ncols_pad != N:
        wp = np.zeros((K, ncols_pad), w.dtype)
        wp[:, :N] = w
        w = wp
        N = ncols_pad
    a = w.reshape(K // 128, 128, N // 128, 128)
    return np.ascontiguousarray(a.transpose(2, 1, 0, 3)).reshape(N // 128, 128, (K // 128) * 128)


def ln_tile(v):
    return np.ascontiguousarray(v.reshape(-1, 128).T)


NEG = -30000.0
HD = 128
ATT_SCALE = HD ** -0.5


def attn_consts(S, slopes_nsa, slopes_moba):
    import ml_dtypes
    bf = ml_dtypes.bfloat16
    NQ = S // 512
    c = {}
    sl = np.concatenate([slopes_nsa, slopes_moba]).astype(np.float64)
    f = np.arange(512, dtype=np.float64)
    cr = -(sl[:, None] * f[None, :]) / ATT_SCALE
    hi = cr.astype(bf).astype(np.float64)
    lo = (cr - hi).astype(bf).astype(np.float64)
    c["crow"] = np.stack([hi, lo], axis=1).astype(np.float32)
    p = np.arange(128, dtype=np.float64)
    r = np.arange(-3, 61, dtype=np.float64)
    c["ab"] = (sl[None, :, None] * (p[:, None, None] - 128.0 * r[None, None, :])).astype(np.float32)
    nt = np.arange(4)
    q = np.arange(NQ)
    c["abc"] = (sl[None, :4, None, None] * (16.0 * (128.0 * nt[None, None, :, None] + p[:, None, None, None]) + 31.0
                                            - 512.0 * q[None, None, None, :])).astype(np.float32).reshape(128, 4, 4 * NQ)
    P = p[:, None]
    Fq = f[None, :]
    masks = []
    for rr in (0, -1, -2, -3):
        masks.append(np.where(P - Fq - 128 * rr <= 0, 0.0, NEG))
    for rr in (1, 2, 3, 4):
        masks.append(np.where(P - Fq - 128 * rr > -512, 0.0, NEG))
    for e in range(5):
        masks.append(np.where(16 * P + 31 - 512 * e <= Fq, 0.0, NEG))
    c["masks"] = np.stack(masks, axis=1).astype(np.float32)
    NSEL = S // 64
    NKT = S // 128
    E = np.zeros((NSEL, NKT, 128), np.float32)
    for kt in range(NKT):
        E[2 * kt, kt, :64] = 1
        E[2 * kt + 1, kt, 64:] = 1
    c["ensa"] = E
    NB = S // 256
    Em = np.zeros((NB + 2, NB, 128), np.float32)
    for b in range(NB):
        Em[b, b, :] = 1
    Em[NB:, :, :] = 1
    c["emoba"] = Em
    ncmp = S // 16 - 1
    n = np.arange(((ncmp + 127) // 128) * 128)
    j = np.arange(NSEL)
    mem = ((16 * n[:, None] < 64 * j[None, :] + 64) & (16 * n[:, None] + 32 > 64 * j[None, :]) & (n[:, None] < ncmp))
    c["member"] = np.ascontiguousarray(mem.astype(np.float32).reshape(-1, 128, NSEL).transpose(1, 0, 2))
    cc = np.arange(256)[None, :]
    hp = (np.arange(128)[:, None] >= 64).astype(np.int64)
    fut = (cc - 128 > hp).astype(np.float32)
    own = ((cc - 128 - hp == 0) | (cc - 128 - hp == -1)).astype(np.float32)
    c["selk"] = np.stack([1.0 - fut, fut, 1e4 * own], axis=1).astype(np.float32)
    c["ident"] = np.eye(128, dtype=np.float32)
    gs = np.zeros((12, 12, 128), np.float32)
    for k in range(12):
        gs[k, k, :] = 1
    c["gsel"] = gs
    return c


def build_attn(S):
    nc = bass.Bass("TRN2", target_bir_lowering=False)
    cx = Ctx(nc)
    NQ = S // 512
    NKT = S // 128
    NSEL = S // 64
    NB = S // 256
    NCMP = S // 16 - 1
    NCT = (NCMP + 127) // 128
    GW = max(NB, 8)

    def din(name, shape):
        return nc.dram_tensor(name, list(shape), F32, kind="ExternalInput").ap()

    nqT = din("nqT", [4, 128, S]); kcT = din("kcT", [128, S]); vcT = din("vcT", [128, S])
    ksT = din("ksT", [128, S]); vs = din("vs", [S, 128]); kwT = din("kwT", [128, S]); vw = din("vw", [S, 128])
    ngT = din("ngT", [12, S])
    mqT = din("mqT", [4, 128, S]); mkT = din("mkT", [4, 128, S]); mv = din("mv", [4, S, 128])
    w1 = [din("w1k", [128, 32 * 128]), din("w1v", [128, 32 * 128])]
    posT = [din("poskT", [128, 32]), din("posvT", [128, 32])]
    w2 = [din("w2k", [128, 128]), din("w2v", [128, 128])]
    crow = din("crow", [8, 2, 512]); ab_d = din("ab", [128, 8, 64]); abc_d = din("abc", [128, 4, 4 * NQ])
    masks_d = din("masks", [128, 13, 512]); ensa_d = din("ensa", [NSEL, NKT, 128]); emoba_d = din("emoba", [NB + 2, NB, 128])
    member_d = din("member", [128, NCT, NSEL]); selk_d = din("selk", [128, 3, 256]); ident_d = din("ident", [128, 128])
    gsel_d = din("gsel", [12, 12, 128])
    oT = nc.dram_tensor("oT", [8 * 128, S], F32, kind="ExternalOutput").ap()
    oacc = nc.dram_tensor("oacc", [4 * 128, S], F32, kind="Internal").ap()
    oT_b = cx.buf("oT"); oacc_b = cx.buf("oacc")

    def sb(name, shape, dt=F32):
        return nc.alloc_sbuf_tensor("s_" + name, list(shape), dt), cx.buf(name)

    QB = [sb("qb%d" % i, [128, S], BF16) for i in range(2)]
    QT = [sb("qt%d" % i, [128, 512], BF16) for i in range(2)]
    KV = [sb("kv%d" % i, [128, S], BF16) for i in range(4)]
    AT, AT_b = sb("AT", [128, max(S, 4096)], BF16)
    EB, EB_b = sb("EB", [128, NKT * 128], BF16)
    masks, masks_b = sb("masks", [128, 13, 512], BF16)
    ab, ab_b = sb("ab", [128, 8, 64]); abc, abc_b = sb("abc", [128, 4, 4 * NQ])
    member, member_b = sb("member", [128, NCT, NSEL], BF16)
    selk, selk_b = sb("selk", [128, 3, 256])
    ident, ident_b = sb("ident", [128, 128], BF16)
    onesb, onesb_b = sb("onesb", [128, 128], BF16)
    gsel, gsel_b = sb("gsel", [12, 12 * 128])
    NGQ = [sb("ngq%d" % i, [12, 512]) for i in range(2)]
    crw, crw_b = sb("crw", [2, 8, 512], BF16)
    pT = [sb("pT%d" % i, [128, 512], BF16) for i in range(4)]
    fA = [sb("fA%d" % i, [128, 512]) for i in range(2)]
    fB = [sb("fB%d" % i, [128, 512]) for i in range(2)]
    fC = [sb("fC%d" % i, [128, 512]) for i in range(2)]
    fD = [sb("fD%d" % i, [128, 512]) for i in range(2)]
    s1, s1_b = sb("s1", [128, 128]); s2, s2_b = sb("s2", [128, 128])
    m8a, m8a_b = sb("m8a", [128, 8]); m8b, m8b_b = sb("m8b", [128, 8])
    mbt, mbt_b = sb("mbt", [128, 128], BF16)
    kcc = [sb("kcc%d" % i, [128, 512], BF16) for i in range(2)]
    hid, hid_b = sb("hid", [128, 512], BF16)
    cb, cb_b = sb("cb", [128, 2])
    pw2 = [sb("w2_%d" % i, [128, 128], BF16) for i in range(2)]
    pposT = [sb("posT%d" % i, [128, 32], BF16) for i in range(2)]
    kmT, kmT_b = sb("kmT", [128, 32]); kmTb, kmTb_b = sb("kmTb", [128, 32], BF16)
    U3, U3_b = sb("U3", [128, 3, 64])
    PS = [(nc.alloc_psum_tensor("ps%d" % i, [128, 512], F32), cx.buf("ps%d" % i)) for i in range(8)]
    ST = PS[0:2]; OACC = PS[2]; RS = PS[3]; IMP = PS[4]; MISC = PS[5:8]
    misc_i = [0]

    def misc():
        misc_i[0] += 1
        return MISC[misc_i[0] % 3]

    def ld(dst, dst_b, src, q="pool"):
        cx.dma(q, dst, src, dst_b, [], [dst_b])

    ld(masks[:, :, :], masks_b, masks_d)
    ld(ab[:, :, :], ab_b, ab_d, "sp"); ld(abc[:, :, :], abc_b, abc_d, "sp")
    ld(member[:, :, :], member_b, member_d); ld(selk[:, :, :], selk_b, selk_d, "sp")
    ld(ident[:, :], ident_b, ident_d); ld(gsel[:, :], gsel_b, gsel_d.rearrange("k r m -> k (r m)"), "sp")
    ld(crw[:, :, :], crw_b, crow.rearrange("h r f -> r h f"))
    cx.op("dve", lambda e: e.memset(onesb[:, :], 1.0), [], [onesb_b])
    cx.op("dve", lambda e: e.memset(U3[:, :, :], 0.0), [], [U3_b])
    cx.op("dve", lambda e: e.memset(U3[:, 0, 0:32], 1.0), [], [U3_b])
    cx.op("dve", lambda e: e.memset(U3[:, 1, 32:33], 1.0), [], [U3_b])
    cx.op("dve", lambda e: e.memset(U3[:, 2, 32:64], -1e30), [], [U3_b])

    def exp_tile(st, st_b, kp, bias_ap, bias_b, slot):
        p, p_b = pT[slot % 4]
        cx.op("act", lambda e: e.activation(out=p[:kp, :], in_=st[:kp, :], func=AF.Exp, bias=bias_ap, scale=ATT_SCALE),
              [st_b, bias_b], [p_b])
        return p, p_b

    cnt = {"pair": 0, "it": 0}

    def attn_pairs(q_rhs, q_b, pairs):
        kept = []
        n = len(pairs)

        def pv(i):
            p, p_b, kp = kept[i]
            pr = pairs[i]
            mm(cx, OACC[0][:, :], pr["v"], p[:kp, :], i == 0, i == n - 1, [pr["v_b"], p_b], [OACC[1]])
            mm(cx, RS[0][:, :], onesb[:kp, :], p[:kp, :], i == 0, i == n - 1, [onesb_b, p_b], [RS[1]])

        for i, pr in enumerate(pairs):
            st, st_b = ST[cnt["pair"] % 2]
            kp = pr["kp"]
            terms = [(pr["kT"], q_rhs, [pr["k_b"], q_b])] + pr["extra"]
            for ti, (l, r, bs) in enumerate(terms):
                mm(cx, st[:kp, :], l, r, ti == 0, ti == len(terms) - 1, bs, [st_b])
            p, p_b = exp_tile(st, st_b, kp, pr["bias"], pr["bias_b"], cnt["pair"])
            cnt["pair"] += 1
            kept.append((p, p_b, kp))
            if i >= 1:
                pv(i - 1)
        pv(n - 1)
        return kept

    def gate_weight(row, Q):
        j = cnt["it"] % 2
        cnt["it"] += 1
        g_ps, g_psb = misc()
        ngq, ngq_b = NGQ[j]
        cx.dma("sp", ngq[:, :], ngT[:, Q * 512:(Q + 1) * 512], ngq_b, [], [ngq_b])
        cx.op("pe", lambda e: e.matmul(g_ps[:, :], gsel[:, row * 128:(row + 1) * 128], ngq[:, :],
                                       start=True, stop=True), [gsel_b, ngq_b], [g_psb])
        cx.op("act", lambda e: e.activation(out=fB[j][0][:, :], in_=g_ps[:, :], func=AF.Sigmoid), [g_psb], [fB[j][1]])
        cx.op("dve", lambda e: e.tensor_scalar(out=fA[j][0][:, :], in0=RS[0][:, :], scalar1=1e-30, scalar2=None,
                                               op0=ALU.max), [RS[1]], [fA[j][1]])
        cx.op("dve", lambda e: e.reciprocal(out=fA[j][0][:, :], in_=fA[j][0][:, :]), [fA[j][1]], [fA[j][1]])
        return j

    def plain_weight():
        j = cnt["it"] % 2
        cnt["it"] += 1
        cx.op("dve", lambda e: e.tensor_scalar(out=fA[j][0][:, :], in0=RS[0][:, :], scalar1=1e-30, scalar2=None,
                                               op0=ALU.max), [RS[1]], [fA[j][1]])
        cx.op("dve", lambda e: e.reciprocal(out=fA[j][0][:, :], in_=fA[j][0][:, :]), [fA[j][1]], [fA[j][1]])
        return j

    kcT_s, kcT_sb = kcc[0]
    vc_s, vc_sb = kcc[1]
    w1s, w1s_b = AT, AT_b
    for which, src in enumerate((kcT, vcT)):
        raw, raw_b = KV[which]
        ld(raw[:, :], raw_b, src)
        ld(w1s[:, 0:4096], w1s_b, w1[which])
        ld(pw2[which][0][:, :], pw2[which][1], w2[which])
        ld(pposT[which][0][:, :], pposT[which][1], posT[which])
        hp, hp_b = misc()
        for r in range(32):
            rhs = raw[:, r:r + 16 * (NCMP - 1) + 1:16]
            mm(cx, hp[:, 0:NCMP], w1s[:, r * 128:(r + 1) * 128], rhs, r == 0, r == 31, [w1s_b, raw_b], [hp_b])
        bp, bp_b = misc()
        for r in range(32):
            mm(cx, bp[:, 0:1], w1s[:, r * 128:(r + 1) * 128], pposT[which][0][:, r:r + 1], r == 0, r == 31,
               [w1s_b, pposT[which][1]], [bp_b])
        cx.op("dve", lambda e: e.tensor_copy(out=cb[:, which:which + 1], in_=bp[:, 0:1]), [bp_b], [cb_b])
        xs, xs_b = fC[0]; x2, x2_b = fC[1]
        cx.op("act", lambda e: e.activation(out=xs[:, 0:NCMP], in_=hp[:, 0:NCMP], func=AF.Identity,
                                            bias=cb[:, which:which + 1], scale=1.0), [hp_b, cb_b], [xs_b])
        cx.op("dve", lambda e: e.tensor_tensor(out=x2[:, 0:NCMP], in0=xs[:, 0:NCMP], in1=xs[:, 0:NCMP], op=ALU.mult),
              [xs_b], [x2_b])
        cx.op("dve", lambda e: e.tensor_scalar(out=x2[:, 0:NCMP], in0=x2[:, 0:NCMP], scalar1=0.044715, scalar2=1.0,
                                               op0=ALU.mult, op1=ALU.add), [x2_b], [x2_b])
        cx.op("dve", lambda e: e.tensor_tensor(out=x2[:, 0:NCMP], in0=x2[:, 0:NCMP], in1=xs[:, 0:NCMP], op=ALU.mult),
              [x2_b, xs_b], [x2_b])
        cx.op("act", lambda e: e.activation(out=x2[:, 0:NCMP], in_=x2[:, 0:NCMP], func=AF.Sigmoid,
                                            scale=2.0 * 0.7978845608028654), [x2_b], [x2_b])
        cx.op("dve", lambda e: e.memset(hid[:, :], 0.0), [], [hid_b])
        cx.op("dve", lambda e: e.tensor_tensor(out=hid[:, 0:NCMP], in0=x2[:, 0:NCMP], in1=xs[:, 0:NCMP], op=ALU.mult),
              [x2_b, xs_b], [hid_b])
        if which == 0:
            op_, op_b = misc()
            mm(cx, op_[:, 0:512], pw2[0][0][:, :], hid[:, :], True, True, [pw2[0][1], hid_b], [op_b])
            cx.op("dve", lambda e: e.tensor_copy(out=kcT_s[:, :], in_=op_[:, 0:512]), [op_b], [kcT_sb])
        else:
            op_, op_b = misc()
            for ntile in range(NCT):
                mm(cx, op_[:, ntile * 128:(ntile + 1) * 128], hid[:, ntile * 128:(ntile + 1) * 128], pw2[1][0][:, :],
                   True, True, [hid_b, pw2[1][1]], [op_b])
            cx.op("dve", lambda e: e.tensor_copy(out=vc_s[:, 0:NCT * 128], in_=op_[:, 0:NCT * 128]), [op_b], [vc_sb])

    ld(EB[0:NSEL, :], EB_b, ensa_d.rearrange("n k p -> n (k p)"))
    AT_written = False
    for Q in range(NQ):
        qs = slice(Q * 512, (Q + 1) * 512)
        nmax = min((512 * Q + 480) // 16, NCMP - 1)
        tiles = list(range(nmax // 128 + 1))
        for h in range(4):
            pairs = []
            for nt_ in tiles:
                kp = min(128, NCMP - nt_ * 128)
                extra = [(onesb[0:2, 0:kp], crw[:, h, :], [onesb_b, crw_b])]
                e_ = Q - 4 * nt_
                if 0 <= e_ <= 4:
                    extra.append((ident[:, 0:kp], masks[:, 8 + e_, :], [ident_b, masks_b]))
                pairs.append(dict(kT=kcT_s[:, nt_ * 128:nt_ * 128 + kp], k_b=kcT_sb, kp=kp, extra=extra,
                                  bias=abc[0:kp, h, nt_ * NQ + Q:nt_ * NQ + Q + 1], bias_b=abc_b,
                                  v=vc_s[0:kp, nt_ * 128:(nt_ + 1) * 128], v_b=vc_sb))
            qt_, qt_b = QT[(Q * 4 + h) % 2]
            ld(qt_[:, :], qt_b, nqT[h][:, qs])
            kept = attn_pairs(qt_[:, :], qt_b, pairs)
            j = gate_weight(h * 3 + 0, Q)
            for ii, (p, p_b, kp) in enumerate(kept):
                cx.op("dve", lambda e: e.tensor_tensor(out=p[:kp, :], in0=p[:kp, :], in1=fA[j][0][:kp, :], op=ALU.mult),
                      [p_b, fA[j][1]], [p_b])
                for ts in range(4):
                    mm(cx, IMP[0][:, ts * 128:ts * 128 + NSEL], p[:kp, ts * 128:(ts + 1) * 128],
                       member[0:kp, tiles[ii], :], h == 0 and ii == 0, h == 3 and ii == len(kept) - 1,
                       [p_b, member_b], [IMP[1]])
            cx.op("dve", lambda e: e.tensor_tensor(out=fA[j][0][:, :], in0=fA[j][0][:, :], in1=fB[j][0][:, :], op=ALU.mult),
                  [fA[j][1], fB[j][1]], [fA[j][1]])
            cx.op("dve", lambda e: e.tensor_tensor(out=fC[j][0][:, :], in0=OACC[0][:, :], in1=fA[j][0][:, :], op=ALU.mult),
                  [OACC[1], fA[j][1]], [fC[j][1]])
            cx.dma("sp", oacc[h * 128:(h + 1) * 128, qs], fC[j][0][:, :], fC[j][1], [fC[j][1]], [oacc_b])
        for ts in range(4):
            tt = Q * 4 + ts
            w0 = 128 - 2 * tt
            if w0 < 0:
                w0 = None
            imp_ap = IMP[0][:, ts * 128:ts * 128 + NSEL]
            c0 = 128 - 2 * tt
            cx.op("dve", lambda e: e.tensor_tensor(out=s1[:, 0:NSEL], in0=imp_ap, in1=selk[:, 0, c0:c0 + NSEL], op=ALU.mult),
                  [IMP[1], selk_b], [s1_b])
            cx.op("dve", lambda e: e.tensor_tensor(out=s1[:, 0:NSEL], in0=s1[:, 0:NSEL], in1=selk[:, 1, c0:c0 + NSEL],
                                                   op=ALU.subtract), [s1_b, selk_b], [s1_b])
            cx.op("dve", lambda e: e.tensor_tensor(out=s1[:, 0:NSEL], in0=s1[:, 0:NSEL], in1=selk[:, 2, c0:c0 + NSEL],
                                                   op=ALU.add), [s1_b, selk_b], [s1_b])
            cx.op("dve", lambda e: e.tensor_scalar(out=s1[:, 0:1], in0=s1[:, 0:1], scalar1=1e4, scalar2=None, op0=ALU.add),
                  [s1_b], [s1_b])
            cx.op("dve", lambda e: e.max(out=m8a[:, :], in_=s1[:, 0:NSEL]), [s1_b], [m8a_b])
            cx.op("dve", lambda e: e.match_replace(out=s2[:, 0:NSEL], in_to_replace=m8a[:, :], in_values=s1[:, 0:NSEL],
                                                   imm_value=-1e30), [m8a_b, s1_b], [s2_b])
            cx.op("dve", lambda e: e.max(out=m8b[:, :], in_=s2[:, 0:NSEL]), [s2_b], [m8b_b])
            cx.op("dve", lambda e: e.tensor_scalar(out=s2[:, 0:NSEL], in0=s1[:, 0:NSEL], scalar1=m8b[:, 7:8], scalar2=None,
                                                   op0=ALU.is_ge), [s1_b, m8b_b], [s2_b])
            cx.op("dve", lambda e: e.tensor_scalar(out=mbt[:, 0:NSEL], in0=s2[:, 0:NSEL], scalar1=-NEG, scalar2=NEG,
                                                   op0=ALU.mult, op1=ALU.add), [s2_b], [mbt_b])
            tp, tp_b = misc()
            mm(cx, tp[0:NSEL, 0:128], mbt[:, 0:NSEL], ident[:, :], True, True, [mbt_b, ident_b], [tp_b])
            cx.op("act", lambda e: e.activation(out=AT[0:NSEL, tt * 128:(tt + 1) * 128], in_=tp[0:NSEL, 0:128], func=AF.Copy),
                  [tp_b], [AT_b])

    ld(KV[0][0][:, :], KV[0][1], ksT)
    ld(KV[1][0][:, :].rearrange("p (k d) -> p k d", d=128), KV[1][1], vs.rearrange("(k p) d -> p k d", p=128))
    ld(KV[2][0][:, :], KV[2][1], kwT)
    ld(KV[3][0][:, :].rearrange("p (k d) -> p k d", d=128), KV[3][1], vw.rearrange("(k p) d -> p k d", p=128))
    for h in range(4):
        ld(QB[h % 2][0][:, :], QB[h % 2][1], nqT[h])
        for Q in range(NQ):
            qs = slice(Q * 512, (Q + 1) * 512)
            q_rhs, q_b = QB[h % 2][0][:, qs], QB[h % 2][1]
            jd = cnt["it"] % 2
            cx.dma("sp", fD[jd][0][:, :], oacc[h * 128:(h + 1) * 128, qs], fD[jd][1], [oacc_b], [fD[jd][1]])
            for br in (1, 2):
                pairs = []
                kts = range(0, 4 * Q + 4) if br == 1 else range(max(0, 4 * Q - 4), 4 * Q + 4)
                kbuf, vbuf = (KV[0], KV[1]) if br == 1 else (KV[2], KV[3])
                for kt in kts:
                    r = 4 * Q - kt
                    extra = [(onesb[0:2, :], crw[:, h, :], [onesb_b, crw_b])]
                    if br == 1:
                        extra.append((EB[0:NSEL, kt * 128:(kt + 1) * 128], AT[0:NSEL, qs], [EB_b, AT_b]))
                    if r <= 0:
                        extra.append((ident[:, :], masks[:, -r, :], [ident_b, masks_b]))
                    elif br == 2:
                        extra.append((ident[:, :], masks[:, 3 + r, :], [ident_b, masks_b]))
                    pairs.append(dict(kT=kbuf[0][:, kt * 128:(kt + 1) * 128], k_b=kbuf[1], kp=128, extra=extra,
                                      bias=ab[:, h, r + 3:r + 4], bias_b=ab_b,
                                      v=vbuf[0][:, kt * 128:(kt + 1) * 128], v_b=vbuf[1]))
                attn_pairs(q_rhs, q_b, pairs)
                j = gate_weight(h * 3 + br, Q)
                cx.op("dve", lambda e: e.tensor_tensor(out=fA[j][0][:, :], in0=fA[j][0][:, :], in1=fB[j][0][:, :],
                                                       op=ALU.mult), [fA[j][1], fB[j][1]], [fA[j][1]])
                cx.op("dve", lambda e: e.tensor_tensor(out=fC[j][0][:, :], in0=OACC[0][:, :], in1=fA[j][0][:, :],
                                                       op=ALU.mult), [OACC[1], fA[j][1]], [fC[j][1]])
                cx.op("dve", lambda e: e.tensor_tensor(out=fD[jd][0][:, :], in0=fD[jd][0][:, :], in1=fC[j][0][:, :],
                                                       op=ALU.add), [fD[jd][1], fC[j][1]], [fD[jd][1]])
            cx.dma("sp", oT[h * 128:(h + 1) * 128, qs], fD[jd][0][:, :], fD[jd][1], [fD[jd][1]], [oT_b])

    ld(EB[0:NB + 2, 0:NB * 128], EB_b, emoba_d.rearrange("n k p -> n (k p)"))
    for h in range(4):
        kb, vb = KV[(h % 2) * 2], KV[(h % 2) * 2 + 1]
        QBh = QB[h % 2]
        ld(QBh[0][:, :], QBh[1], mqT[h])
        ld(kb[0][:, :], kb[1], mkT[h])
        ld(vb[0][:, :].rearrange("p (k d) -> p k d", d=128), vb[1], mv[h].rearrange("(k p) d -> p k d", p=128))
        hh = 4 + h
        for Q in range(NQ):
            cx.dma("pool", AT[NB:NB + 2, Q * 512:(Q + 1) * 512], crow[hh], AT_b, [], [AT_b])
        cx.op("dve", lambda e: e.tensor_reduce(out=kmT[:, 0:NB], in_=kb[0][:, :].rearrange("p (n l) -> p n l", l=256),
                                               axis=AX.X, op=ALU.add), [kb[1]], [kmT_b])
        cx.op("dve", lambda e: e.tensor_scalar(out=kmTb[:, 0:NB], in0=kmT[:, 0:NB], scalar1=1.0 / 256, scalar2=None,
                                               op0=ALU.mult), [kmT_b], [kmTb_b])
        for tt in range(NKT):
            own = tt // 2
            gp, gp_b = misc()
            mm(cx, gp[:, 0:NB], QBh[0][:, tt * 128:(tt + 1) * 128], kmTb[:, 0:NB], True, True, [QBh[1], kmTb_b], [gp_b])
            if GW > NB:
                cx.op("dve", lambda e: e.memset(s1[:, 0:GW], -1e30), [], [s1_b])
            cx.op("dve", lambda e: e.tensor_tensor(out=s1[:, 0:NB], in0=gp[:, 0:NB], in1=U3[:, 2, 32 - own:32 - own + NB],
                                                   op=ALU.add), [gp_b, U3_b], [s1_b])
            cx.op("dve", lambda e: e.max(out=m8a[:, :], in_=s1[:, 0:GW]), [s1_b], [m8a_b])
            cx.op("dve", lambda e: e.tensor_scalar(out=s2[:, 0:NB], in0=s1[:, 0:NB], scalar1=m8a[:, 2:3], scalar2=None,
                                                   op0=ALU.is_ge), [s1_b, m8a_b], [s2_b])
            cx.op("dve", lambda e: e.tensor_tensor(out=s2[:, 0:NB], in0=s2[:, 0:NB], in1=U3[:, 0, 32 - own:32 - own + NB],
                                                   op=ALU.mult), [s2_b, U3_b], [s2_b])
            cx.op("dve", lambda e: e.tensor_tensor(out=s2[:, 0:NB], in0=s2[:, 0:NB], in1=U3[:, 1, 32 - own:32 - own + NB],
                                                   op=ALU.max), [s2_b, U3_b], [s2_b])
            cx.op("dve", lambda e: e.tensor_scalar(out=mbt[:, 0:NB], in0=s2[:, 0:NB], scalar1=-NEG, scalar2=NEG,
                                                   op0=ALU.mult, op1=ALU.add), [s2_b], [mbt_b])
            tp, tp_b = misc()
            mm(cx, tp[0:NB, 0:128], mbt[:, 0:NB], ident[:, :], True, True, [mbt_b, ident_b], [tp_b])
            cx.op("act", lambda e: e.activation(out=AT[0:NB, tt * 128:(tt + 1) * 128], in_=tp[0:NB, 0:128], func=AF.Copy),
                  [tp_b], [AT_b])
        for Q in range(NQ):
            qs = slice(Q * 512, (Q + 1) * 512)
            pairs = []
            for kt in range(0, 4 * Q + 4):
                r = 4 * Q - kt
                blk = kt // 2
                extra = [(EB[0:NB + 2, blk * 128:(blk + 1) * 128], AT[0:NB + 2, qs], [EB_b, AT_b])]
                if r <= 0:
                    extra.append((ident[:, :], masks[:, -r, :], [ident_b, masks_b]))
                pairs.append(dict(kT=kb[0][:, kt * 128:(kt + 1) * 128], k_b=kb[1], kp=128, extra=extra,
                                  bias=ab[:, hh, r + 3:r + 4], bias_b=ab_b,
                                  v=vb[0][:, kt * 128:(kt + 1) * 128], v_b=vb[1]))
            attn_pairs(QBh[0][:, qs], QBh[1], pairs)
            j = plain_weight()
            cx.op("dve", lambda e: e.tensor_tensor(out=fC[j][0][:, :], in0=OACC[0][:, :], in1=fA[j][0][:, :], op=ALU.mult),
                  [OACC[1], fA[j][1]], [fC[j][1]])
            cx.dma("sp", oT[(4 + h) * 128:(5 + h) * 128, qs], fC[j][0][:, :], fC[j][1], [fC[j][1]], [oT_b])
    cx.finish([oT_b])
    return nc


def attn_inputs(nq, kc, vc, ks, vs, kw, vw, ng, mq, mk, mv):
    C = np.ascontiguousarray
    return {"nqT": C(nq.transpose(1, 2, 0)), "kcT": C(kc.T), "vcT": C(vc.T), "ksT": C(ks.T), "vs": C(vs),
            "kwT": C(kw.T), "vw": C(vw), "ngT": C(ng.T), "mqT": C(mq.transpose(1, 2, 0)),
            "mkT": C(mk.transpose(1, 2, 0)), "mv": C(mv.transpose(1, 0, 2))}


def attn_weights(posk, w1k, w2k, posv, w1v, w2v):
    C = np.ascontiguousarray
    t1 = lambda w: C(w.reshape(32, 128, 128).transpose(1, 0, 2)).reshape(128, 32 * 128)
    return {"w1k": t1(w1k), "w1v": t1(w1v), "poskT": C(posk.T), "posvT": C(posv.T), "w2k": C(w2k), "w2v": C(w2v)}


GLA_TAU = 16.0
NORM_EPS = 1e-6


def build_mixer(S):
    nc = bass.Bass("TRN2", target_bir_lowering=False)
    cx = Ctx(nc)
    NT = S // 128
    NQ = S // 512

    def din(name, shape):
        return nc.dram_tensor(name, list(shape), F32, kind="ExternalInput").ap()

    gbT = din("gbT", [512, S]); gcT = din("gcT", [512, S]); hT = din("hT", [512, S]); cw_d = din("cw", [128, 4, 3])
    gqT = din("gqT", [2, 128, S]); gkT = din("gkT", [2, 128, S]); gk = din("gk", [S, 256]); gv = din("gv", [S, 512])
    gg = din("gg", [S, 512]); zaT = din("zaT", [16, S]); wa_d = din("wa", [17, 256]); ngb_d = din("ngb", [128, 512])
    tri_d = din("tri", [128, 2, 128])
    ycT = nc.dram_tensor("ycT", [512, S], F32, kind="ExternalOutput").ap()
    yg = nc.dram_tensor("yg", [S, 512], F32, kind="ExternalOutput").ap()
    ycT_b = cx.buf("ycT"); yg_b = cx.buf("yg")

    def sb(name, shape, dt=F32):
        return nc.alloc_sbuf_tensor("s_" + name, list(shape), dt), cx.buf(name)

    def sb2(name, shape, dt=F32):
        return [sb("%s%d" % (name, i), shape, dt) for i in range(2)]

    PS = [(nc.alloc_psum_tensor("ps%d" % i, [128, 512], F32), cx.buf("ps%d" % i)) for i in range(8)]
    pi = [0]

    def psum():
        pi[0] += 1
        return PS[2 + pi[0] % 6]

    cw, cw_b = sb("cw", [128, 4, 3])
    cx.dma("sp", cw[:, :, :], cw_d, cw_b, [], [cw_b])
    U = sb2("cu", [128, 514]); Hh = sb2("ch", [128, 514]); Gb = sb2("cgb", [128, 512]); Ac = sb2("cacc", [128, 512])
    it = 0
    for Q in range(NQ):
        t0 = Q * 512
        for c in range(4):
            j = it % 2
            it += 1
            u, u_b = U[j]; hh, hh_b = Hh[j]; gb_, gb_b = Gb[j]; ac, ac_b = Ac[j]
            rows = slice(c * 128, (c + 1) * 128)
            if Q == 0:
                cx.op("dve", lambda e: e.memset(u[:, 0:2], 0.0), [], [u_b])
                cx.op("dve", lambda e: e.memset(hh[:, 0:2], 0.0), [], [hh_b])
                cx.dma("sp", u[:, 2:514], gcT[rows, 0:512], u_b, [], [u_b])
                cx.dma("sp", hh[:, 2:514], hT[rows, 0:512], hh_b, [], [hh_b])
            else:
                cx.dma("sp", u[:, :], gcT[rows, t0 - 2:t0 + 512], u_b, [], [u_b])
                cx.dma("sp", hh[:, :], hT[rows, t0 - 2:t0 + 512], hh_b, [], [hh_b])
            cx.dma("sp", gb_[:, :], gbT[rows, t0:t0 + 512], gb_b, [], [gb_b])
            cx.op("dve", lambda e: e.tensor_tensor(out=u[:, :], in0=u[:, :], in1=hh[:, :], op=ALU.mult), [u_b, hh_b], [u_b])
            cx.op("dve", lambda e: e.tensor_scalar(out=ac[:, :], in0=u[:, 2:514], scalar1=cw[:, c, 2:3], scalar2=None,
                                                   op0=ALU.mult), [u_b, cw_b], [ac_b])
            cx.op("dve", lambda e: e.scalar_tensor_tensor(out=ac[:, :], in0=u[:, 1:513], scalar=cw[:, c, 1:2], in1=ac[:, :],
                                                          op0=ALU.mult, op1=ALU.add), [u_b, cw_b, ac_b], [ac_b])
            cx.op("dve", lambda e: e.scalar_tensor_tensor(out=ac[:, :], in0=u[:, 0:512], scalar=cw[:, c, 0:1], in1=ac[:, :],
                                                          op0=ALU.mult, op1=ALU.add), [u_b, cw_b, ac_b], [ac_b])
            cx.op("dve", lambda e: e.tensor_tensor(out=ac[:, :], in0=ac[:, :], in1=gb_[:, :], op=ALU.mult), [ac_b, gb_b], [ac_b])
            cx.dma("sp", ycT[rows, t0:t0 + 512], ac[:, :], ac_b, [ac_b], [ycT_b])

    wa, wa_b = sb("wa", [17, 256]); ngb, ngb_b = sb("ngb", [128, 512]); tri, tri_b = sb("tri", [128, 2, 128])
    trib, trib_b = sb("trib", [128, 128])
    cx.dma("sp", wa[:, :], wa_d, wa_b, [], [wa_b])
    cx.dma("sp", ngb[:, :], ngb_d, ngb_b, [], [ngb_b])
    cx.dma("sp", tri[:, :, :], tri_d, tri_b, [], [tri_b])
    Sst = [sb("S%d" % i, [128, 256]) for i in range(2)]
    Sbf = [sb("Sb%d" % i, [128, 256], BF16) for i in range(2)]
    for i in range(2):
        cx.op("dve", lambda e: e.memset(Sst[i][0][:, :], 0.0), [], [Sst[i][1]])
        cx.op("dve", lambda e: e.memset(Sbf[i][0][:, :], 0.0), [], [Sbf[i][1]])
    ZA = sb2("za", [17, 128]); SP = sb2("sp", [128, 256]); KD = sb2("kd", [128, 256], BF16); KDEC = sb2("kdec", [128, 256])
    KTM = sb2("ktm", [128, 256]); VF = sb2("vf", [128, 512]); VB = sb2("vb", [128, 512], BF16); GG = sb2("gg", [128, 512])
    QF = [sb2("qf%d" % p, [128, 128]) for p in range(2)]; KF = [sb2("kf%d" % p, [128, 128]) for p in range(2)]
    EQ = [sb2("eq%d" % p, [128, 128]) for p in range(2)]; EK = [sb2("ek%d" % p, [128, 128]) for p in range(2)]
    QT = [sb2("qtb%d" % p, [128, 128], BF16) for p in range(2)]; KT = [sb2("ktb%d" % p, [128, 128], BF16) for p in range(2)]
    DEC = [sb2("dec%d" % p, [128, 1]) for p in range(2)]
    ATM = [sb("atm%d" % i, [128, 128], BF16) for i in range(4)]
    OSQ = sb2("osq", [128, 512]); SS = sb2("ss", [128, 4]); Y = sb2("y", [128, 512])
    for j in range(2):
        cx.op("dve", lambda e: e.memset(ZA[j][0][0:1, :], 1.0), [], [ZA[j][1]])
    for t in range(NT):
        j = t % 2
        ts = slice(t * 128, (t + 1) * 128)
        za, za_b = ZA[j]
        cx.dma("sp", za[1:17, :], zaT[:, ts], za_b, [], [za_b])
        ktm, ktm_b = KTM[j]; vf, vf_b = VF[j]; vb, vb_b = VB[j]; ggt, gg_b = GG[j]
        cx.dma("sp", ktm[:, :], gk[ts, :], ktm_b, [], [ktm_b])
        cx.dma("sp", vf[:, :], gv[ts, :], vf_b, [], [vf_b])
        cx.dma("sp", ggt[:, :], gg[ts, :], gg_b, [], [gg_b])
        cx.op("act", lambda e: e.activation(out=vb[:, :], in_=vf[:, :], func=AF.Copy), [vf_b], [vb_b])
        zp, zp_b = psum()
        mm(cx, zp[:, 0:256], za[:, :], wa[:, :], True, True, [za_b, wa_b], [zp_b])
        sp_, sp_b = SP[j]
        cx.op("act", lambda e: e.activation(out=sp_[:, :], in_=zp[:, 0:256], func=AF.Exp, scale=-1.0), [zp_b], [sp_b])
        cx.op("act", lambda e: e.activation(out=sp_[:, :], in_=sp_[:, :], func=AF.Ln, bias=1.0, scale=1.0), [sp_b], [sp_b])
        dp, dp_b = psum()
        mm(cx, dp[:, 0:256], tri[:, 1, :], sp_[:, :], True, True, [tri_b, sp_b], [dp_b])
        kdec, kdec_b = KDEC[j]; kd, kd_b = KD[j]
        cx.op("act", lambda e: e.activation(out=kdec[:, :], in_=dp[:, 0:256], func=AF.Exp, scale=-1.0 / GLA_TAU),
              [dp_b], [kdec_b])
        cx.op("dve", lambda e: e.tensor_tensor(out=kd[:, :], in0=ktm[:, :], in1=kdec[:, :], op=ALU.mult),
              [ktm_b, kdec_b], [kd_b])
        op_, op_b = PS[t % 2]
        for pr in range(2):
            qf, qf_b = QF[pr][j]; kf, kf_b = KF[pr][j]
            cx.dma("sp", qf[:, :], gqT[pr][:, ts], qf_b, [], [qf_b])
            cx.dma("sp", kf[:, :], gkT[pr][:, ts], kf_b, [], [kf_b])
            bp, bp_b = psum()
            mm(cx, bp[:, 0:128], sp_[:, pr * 128:(pr + 1) * 128], tri[:, 0, :], True, True, [sp_b, tri_b], [bp_b])
            eq, eq_b = EQ[pr][j]; ek, ek_b = EK[pr][j]; qt, qt_b = QT[pr][j]; kt, kt_b = KT[pr][j]; dec, dec_b = DEC[pr][j]
            cx.op("act", lambda e: e.activation(out=eq[:, :], in_=bp[:, 0:128], func=AF.Exp, scale=-1.0 / GLA_TAU,
                                                bias=math.log(0.125)), [bp_b], [eq_b])
            cx.op("act", lambda e: e.activation(out=ek[:, :], in_=bp[:, 0:128], func=AF.Exp, scale=1.0 / GLA_TAU),
                  [bp_b], [ek_b])
            cx.op("act", lambda e: e.activation(out=dec[:, :], in_=bp[:, 127:128], func=AF.Exp, scale=-1.0 / GLA_TAU),
                  [bp_b], [dec_b])
            cx.op("dve", lambda e: e.tensor_tensor(out=qt[:, :], in0=qf[:, :], in1=eq[:, :], op=ALU.mult), [qf_b, eq_b], [qt_b])
            cx.op("dve", lambda e: e.tensor_tensor(out=kt[:, :], in0=kf[:, :], in1=ek[:, :], op=ALU.mult), [kf_b, ek_b], [kt_b])
            for hf in range(2):
                h = pr * 2 + hf
                prt = slice(hf * 64, (hf + 1) * 64)
                ap_, ap_b = psum()
                mm(cx, ap_[:, 0:128], kt[prt, :], qt[prt, :], True, True, [kt_b, qt_b], [ap_b])
                atm, atm_b = ATM[h]
                cx.op("dve", lambda e: e.tensor_tensor(out=atm[:, :], in0=ap_[:, 0:128], in1=tri[:, 0, :], op=ALU.mult),
                      [ap_b, tri_b], [atm_b])
                mm(cx, op_[:, h * 128:(h + 1) * 128], atm[:, :], vb[:, h * 128:(h + 1) * 128], True, False,
                   [atm_b, vb_b], [op_b])
                mm(cx, op_[:, h * 128:(h + 1) * 128], qt[prt, :], Sbf[pr][0][prt, hf * 128:(hf + 1) * 128], False, True,
                   [qt_b, Sbf[pr][1]], [op_b])
            up, up_b = psum()
            mm(cx, up[:, 0:256], kd[:, pr * 128:(pr + 1) * 128], vb[:, pr * 256:(pr + 1) * 256], True, True,
               [kd_b, vb_b], [up_b])
            cx.op("dve", lambda e: e.scalar_tensor_tensor(out=Sst[pr][0][:, :], in0=Sst[pr][0][:, :], scalar=dec[:, 0:1],
                                                          in1=up[:, 0:256], op0=ALU.mult, op1=ALU.add),
                  [Sst[pr][1], dec_b, up_b], [Sst[pr][1]])
            cx.op("act", lambda e: e.activation(out=Sbf[pr][0][:, :], in_=Sst[pr][0][:, :], func=AF.Copy),
                  [Sst[pr][1]], [Sbf[pr][1]])
        osq, osq_b = OSQ[j]; ss, ss_b = SS[j]; y, y_b = Y[j]
        cx.op("act", lambda e: e.activation(out=osq[:, :], in_=op_[:, :], func=AF.Square), [op_b], [osq_b])
        cx.op("dve", lambda e: e.tensor_reduce(out=ss[:, :], in_=osq[:, :].rearrange("p (h d) -> p h d", d=128), axis=AX.X,
                                               op=ALU.add), [osq_b], [ss_b])
        cx.op("dve", lambda e: e.tensor_scalar(out=ss[:, :], in0=ss[:, :], scalar1=1.0 / 128, scalar2=NORM_EPS,
                                               op0=ALU.mult, op1=ALU.add), [ss_b], [ss_b])
        cx.op("act", lambda e: e.activation(out=ss[:, :], in_=ss[:, :], func=AF.Sqrt), [ss_b], [ss_b])
        cx.op("dve", lambda e: e.reciprocal(out=ss[:, :], in_=ss[:, :]), [ss_b], [ss_b])
        for h in range(4):
            cx.op("dve", lambda e: e.tensor_scalar(out=y[:, h * 128:(h + 1) * 128], in0=op_[:, h * 128:(h + 1) * 128],
                                                   scalar1=ss[:, h:h + 1], scalar2=None, op0=ALU.mult),
                  [op_b, ss_b], [y_b])
        cx.op("dve", lambda e: e.tensor_tensor(out=y[:, :], in0=y[:, :], in1=ngb[:, :], op=ALU.mult), [y_b, ngb_b], [y_b])
        cx.op("act", lambda e: e.activation(out=ggt[:, :], in_=ggt[:, :], func=AF.Silu), [gg_b], [gg_b])
        cx.op("dve", lambda e: e.tensor_tensor(out=y[:, :], in0=y[:, :], in1=ggt[:, :], op=ALU.mult), [y_b, gg_b], [y_b])
        cx.dma("sp", yg[ts, :], y[:, :], y_b, [y_b], [yg_b])
    cx.finish([ycT_b, yg_b])
    return nc


def mixer_inputs(gb, gc, hh, q, k, v, g, za, conv_w, w_a2, b_a, norm_g):
    C = np.ascontiguousarray
    j = np.arange(128)
    tri = np.stack([(j[:, None] <= j[None, :]), (j[:, None] > j[None, :])], axis=1).astype(np.float32)
    return {"gbT": C(gb.T), "gcT": C(gc.T), "hT": C(hh.T), "cw": C(conv_w.T.reshape(4, 128, 3).transpose(1, 0, 2)),
            "gqT": C(q.T.reshape(2, 128, -1)), "gkT": C(k.T.reshape(2, 128, -1)), "gk": C(k), "gv": C(v), "gg": C(g),
            "zaT": C(za.T), "wa": C(np.concatenate([b_a[None, :], w_a2], axis=0)),
            "ngb": C(np.tile(norm_g[None, :], (128, 4))), "tri": tri}


_PROGS = {}


def _prog(key, builder):
    if key not in _PROGS:
        _PROGS[key] = builder()
    return _PROGS[key]


def _run(nc, in_maps):
    res = run_bass_kernel_spmd(nc, in_maps, core_ids=list(range(NCORES)))
    return res.results


T_CORE = BATCH * SEQ // NCORES
L0_SPL = (2048, 512, 512, 512, 512, 512, 512, 48, 2048, 2048, 2048)
L1_SPL = (2048, 2048, 2048, 1024, 1024, 2048, 2048, 16)


def _tok_shards_T(a):
    return [np.ascontiguousarray(a[c * T_CORE:(c + 1) * T_CORE].T) for c in range(NCORES)]


def _gather_T(results, key="yT", ncols=None):
    out = np.concatenate([r[key].T for r in results], axis=0)
    return out if ncols is None else out[:, :ncols]


def _ffn(xf, wg, wu, wd, g, b):
    nc = _prog("ffn", lambda: build_dense("ffn", D_MODEL, D_FF, T_CORE, 512))
    shared = {"wg": tile_w(wg), "wu": tile_w(wu), "wd": tile_w(wd), "lng": ln_tile(g), "lnb": ln_tile(b)}
    xs = _tok_shards_T(xf)
    return _gather_T(_run(nc, [dict(shared, xT=xs[c]) for c in range(NCORES)]))


def _lin(xf, w):
    n = w.shape[1]
    npad = ((n + 127) // 128) * 128
    nc = _prog("lin%d" % npad, lambda: build_dense("lin", D_MODEL, 0, T_CORE, 512, NOUT=npad))
    shared = {"wl": tile_w(w, npad)}
    xs = _tok_shards_T(xf)
    return _gather_T(_run(nc, [dict(shared, xT=xs[c]) for c in range(NCORES)]), ncols=n)


def _outp(xf, of, w, g, b):
    nc = _prog("out", lambda: build_dense("out", D_MODEL, 0, T_CORE, 512))
    shared = {"wd": tile_w(w), "lng": ln_tile(g), "lnb": ln_tile(b)}
    xs = _tok_shards_T(xf)
    os_ = _tok_shards_T(of)
    return _gather_T(_run(nc, [dict(shared, xT=xs[c], oT=os_[c]) for c in range(NCORES)]))


def _split(z, sizes):
    idx = np.cumsum(sizes)[:-1]
    return np.split(z, idx, axis=-1)


def _attn_core(z, pos_k, w1_k, w2_k, pos_v, w1_v, w2_v):
    nc = _prog("attn", lambda: build_attn(SEQ))
    nq, kc, vc, ks, vs, kw, vw, ng, mq, mk, mv = _split(z, L0_SPL)
    slopes = 2.0 ** (-8.0 * np.arange(1, 33, dtype=np.float64) / 32)
    sn = slopes[0::2].reshape(4, 4)
    sm = slopes[1::2]
    wts = attn_weights(pos_k, w1_k, w2_k, pos_v, w1_v, w2_v)
    in_maps = []
    for c in range(NCORES):
        b, g = divmod(c, 4)
        r = slice(b * SEQ, (b + 1) * SEQ)
        s5, s1 = slice(g * 512, (g + 1) * 512), slice(g * 128, (g + 1) * 128)
        im = attn_inputs(nq[r, s5].reshape(SEQ, 4, 128), kc[r, s1], vc[r, s1], ks[r, s1], vs[r, s1], kw[r, s1], vw[r, s1],
                         ng[r, g * 12:(g + 1) * 12], mq[r, s5].reshape(SEQ, 4, 128), mk[r, s5].reshape(SEQ, 4, 128),
                         mv[r, s5].reshape(SEQ, 4, 128))
        im.update(wts)
        im.update(attn_consts(SEQ, sn[g], sm[4 * g:4 * g + 4]))
        in_maps.append(im)
    res = _run(nc, in_maps)
    o = np.empty((BATCH * SEQ, 4096), np.float32)
    for c in range(NCORES):
        b, g = divmod(c, 4)
        oT = res[c]["oT"]
        o[b * SEQ:(b + 1) * SEQ, g * 512:(g + 1) * 512] = oT[0:512].T
        o[b * SEQ:(b + 1) * SEQ, 2048 + g * 512:2048 + (g + 1) * 512] = oT[512:1024].T
    return o


def _mixer_core(z, conv_w, w_a2, b_a, norm_g):
    nc = _prog("mixer", lambda: build_mixer(SEQ))
    gb, gc, hh, q, k, v, g_, za = _split(z, L1_SPL)
    in_maps = []
    for c in range(NCORES):
        b, pt = divmod(c, 4)
        r = slice(b * SEQ, (b + 1) * SEQ)
        s5, s2 = slice(pt * 512, (pt + 1) * 512), slice(pt * 256, (pt + 1) * 256)
        in_maps.append(mixer_inputs(gb[r, s5], gc[r, s5], hh[r, s5], q[r, s2], k[r, s2], v[r, s5], g_[r, s5], za[r],
                                    conv_w[:, s5], w_a2[:, s2], b_a[s2], norm_g))
    res = _run(nc, in_maps)
    m = np.empty((BATCH * SEQ, 4096), np.float32)
    for c in range(NCORES):
        b, pt = divmod(c, 4)
        m[b * SEQ:(b + 1) * SEQ, pt * 512:(pt + 1) * 512] = res[c]["ycT"].T
        m[b * SEQ:(b + 1) * SEQ, 2048 + pt * 512:2048 + (pt + 1) * 512] = res[c]["yg"]
    return m


def kernel(x, ln_g, ln_b, ffn_pre_wg, ffn_pre_wu, ffn_pre_wd, ffn_post_wg, ffn_post_wu, ffn_post_wd,
           att_w_in, att_w_out, nsa_pos_k, nsa_w1_k, nsa_w2_k, nsa_pos_v, nsa_w1_v, nsa_w2_v,
           mix_w_in, mix_w_out, conv_w, gla_w_a2, gla_b_a, gla_norm_g):
    f = lambda a: np.asarray(a, dtype=np.float32)
    xf = f(x).reshape(BATCH * SEQ, D_MODEL)
    ln_g, ln_b = f(ln_g), f(ln_b)
    xf = _ffn(xf, f(ffn_pre_wg)[0], f(ffn_pre_wu)[0], f(ffn_pre_wd)[0], ln_g[0, 0], ln_b[0, 0])
    z = _lin(xf, f(att_w_in)[0])
    o = _attn_core(z, f(nsa_pos_k)[0], f(nsa_w1_k)[0], f(nsa_w2_k)[0], f(nsa_pos_v)[0], f(nsa_w1_v)[0], f(nsa_w2_v)[0])
    del z
    xf = _outp(xf, o, f(att_w_out)[0], ln_g[0, 1], ln_b[0, 1])
    xf = _ffn(xf, f(ffn_post_wg)[0], f(ffn_post_wu)[0], f(ffn_post_wd)[0], ln_g[0, 2], ln_b[0, 2])
    xf = _ffn(xf, f(ffn_pre_wg)[1], f(ffn_pre_wu)[1], f(ffn_pre_wd)[1], ln_g[1, 0], ln_b[1, 0])
    z = _lin(xf, f(mix_w_in)[0])
    m = _mixer_core(z, f(conv_w)[0], f(gla_w_a2)[0], f(gla_b_a)[0], f(gla_norm_g)[0])
    del z
    xf = _outp(xf, m, f(mix_w_out)[0], ln_g[1, 1], ln_b[1, 1])
    xf = _ffn(xf, f(ffn_post_wg)[1], f(ffn_post_wu)[1], f(ffn_post_wd)[1], ln_g[1, 2], ln_b[1, 2])
    return xf.reshape(BATCH, SEQ, D_MODEL)
```
